# Optimizing a Trainium2 kernel written in Bass

```python
import jax, jax.numpy as jnp
from jax import lax
import numpy as np

D_MODEL = 4096
BATCH = 4
SEQ = 4096
DEPTH = 1

D_LRU = D_MODEL // 2
N_LRU_BLOCKS = 16
LRU_BLOCK = D_LRU // N_LRU_BLOCKS
LRU_C = 8.0
CONV_LRU = 4
HEAD_DIM = 128
N_HEADS = (D_MODEL // 2) // HEAD_DIM
N_KV_HEADS = 4
D_ATTN = N_HEADS * HEAD_DIM
D_KV = N_KV_HEADS * HEAD_DIM
D_MIX = D_LRU + D_ATTN
N_IDX_HEADS = 32
IDX_DIM = 64
TOPK_MAX = 256
Q_BLOCK = 128
D_FF = 11008
CONV_FFN = 3
RMS_EPS = 1e-6
N_MOD = 6

SZ_LRU_X = D_LRU
SZ_LRU_GATE = D_LRU
SZ_Q = D_ATTN
SZ_K = D_KV
SZ_V = D_KV
SZ_QIDX = N_IDX_HEADS * IDX_DIM
SZ_KIDX = IDX_DIM
SZ_WIDX = N_IDX_HEADS
D_IN = SZ_LRU_X + SZ_LRU_GATE + SZ_Q + SZ_K + SZ_V + SZ_QIDX + SZ_KIDX + SZ_WIDX
OFF_1 = SZ_LRU_X
OFF_2 = OFF_1 + SZ_LRU_GATE
OFF_3 = OFF_2 + SZ_Q
OFF_4 = OFF_3 + SZ_K
OFF_5 = OFF_4 + SZ_V
OFF_6 = OFF_5 + SZ_QIDX
OFF_7 = OFF_6 + SZ_KIDX

kernel_name = "hymba_rglru_dsa_convffn_adaln"


def rmsnorm(x, g):
    xf = x.astype(jnp.float32)
    y = xf * lax.rsqrt(jnp.mean(xf * xf, axis=-1, keepdims=True) + RMS_EPS)
    return (y * g.astype(jnp.float32)).astype(x.dtype)


def causal_dwconv(x, w, b):
    k_w = w.shape[0]
    s = x.shape[1]
    xp = jnp.pad(x, ((0, 0), (k_w - 1, 0), (0, 0)))
    y = b
    for j in range(k_w):
        y = y + w[j] * xp[:, j:j + s]
    return y


def rg_lru(x, w_a, b_a, w_x, b_x, lam):
    f32 = jnp.float32
    bsz, s, _ = x.shape
    xf = x.astype(f32)
    xb = xf.reshape(bsz, s, N_LRU_BLOCKS, LRU_BLOCK)
    r = jax.nn.sigmoid(jnp.einsum('bsni,nij->bsnj', xb, w_a.astype(f32)).reshape(bsz, s, D_LRU) + b_a.astype(f32))
    i = jax.nn.sigmoid(jnp.einsum('bsni,nij->bsnj', xb, w_x.astype(f32)).reshape(bsz, s, D_LRU) + b_x.astype(f32))
    log_a = -LRU_C * jax.nn.softplus(-lam.astype(f32)) * r
    a = jnp.exp(log_a)
    u = jnp.sqrt(-jnp.expm1(2.0 * log_a)) * (i * xf)

    def combine(e1, e2):
        a1, b1 = e1
        a2, b2 = e2
        return a1 * a2, a2 * b1 + b2

    _, h = lax.associative_scan(combine, (a, u), axis=1)
    return h.astype(x.dtype)


def dsa_attention(q, k, v, q_idx, k_idx, w_idx):
    f32 = jnp.float32
    bsz, s = q.shape[0], q.shape[1]
    n_blk = s // Q_BLOCK
    k_sel = min(TOPK_MAX, s // 4)
    rep = N_HEADS // N_KV_HEADS
    scale = HEAD_DIM ** -0.5
    k_idx_f = k_idx.astype(f32)
    key_pos = jnp.arange(s)

    def to_blocks(t):
        return jnp.swapaxes(t.reshape((bsz, n_blk, Q_BLOCK) + t.shape[2:]), 0, 1)

    def gather_rows(kb, ib):
        return kb[ib]

    def block(args):
        qb, qib, wib, t0 = args
        qpos = t0 + jnp.arange(Q_BLOCK)
        causal = key_pos[None, :] <= qpos[:, None]
        dots = jnp.einsum('bqhd,bsd->bqhs', qib.astype(f32), k_idx_f)
        score = jnp.einsum('bqhs,bqh->bqs', jax.nn.relu(dots), wib.astype(f32))
        score = jnp.where(causal[None], score, -jnp.inf)
        _, sel = lax.top_k(score, k_sel)
        valid = sel <= qpos[None, :, None]
        kg = jax.vmap(gather_rows)(k, sel)
        vg = jax.vmap(gather_rows)(v, sel)
        qg = qb.reshape(bsz, Q_BLOCK, N_KV_HEADS, rep, HEAD_DIM).astype(f32)
        logits = jnp.einsum('bqgrd,bqkgd->bqgrk', qg, kg.astype(f32)) * scale
        logits = jnp.where(valid[:, :, None, None, :], logits, -jnp.inf)
        p = jax.nn.softmax(logits, axis=-1)
        o = jnp.einsum('bqgrk,bqkgd->bqgrd', p, vg.astype(f32))
        return o.reshape(bsz, Q_BLOCK, D_ATTN).astype(q.dtype)

    t0s = jnp.arange(n_blk) * Q_BLOCK
    out = lax.map(block, (to_blocks(q), to_blocks(q_idx), to_blocks(w_idx), t0s))
    return jnp.swapaxes(out, 0, 1).reshape(bsz, s, D_ATTN)


def hybrid_layer(x, c, w_ada, b_ada, g_pre_mix, g_post_mix, g_pre_ffn, g_post_ffn, w_in,
                 conv_lru_w, conv_lru_b, w_rg_a, b_rg_a, w_rg_x, b_rg_x, lru_lambda,
                 g_grp_lru, g_grp_attn, w_out, w_ffn_in, conv_ffn_w, conv_ffn_b, w_ffn_out):
    bsz, s, _ = x.shape
    mod = (jax.nn.silu(c) @ w_ada + b_ada)[:, None, :]
    sh1, sc1, gt1, sh2, sc2, gt2 = jnp.split(mod, N_MOD, axis=-1)

    h = rmsnorm(x, g_pre_mix) * (1.0 + sc1) + sh1
    proj = h @ w_in
    x_lru, gate_lru, q, k, v, q_idx, k_idx, w_idx = jnp.split(
        proj, [OFF_1, OFF_2, OFF_3, OFF_4, OFF_5, OFF_6, OFF_7], axis=-1)

    x_lru = causal_dwconv(x_lru, conv_lru_w, conv_lru_b)
    y_lru = rg_lru(x_lru, w_rg_a, b_rg_a, w_rg_x, b_rg_x, lru_lambda) * jax.nn.gelu(gate_lru)

    y_attn = dsa_attention(q.reshape(bsz, s, N_HEADS, HEAD_DIM),
                           k.reshape(bsz, s, N_KV_HEADS, HEAD_DIM),
                           v.reshape(bsz, s, N_KV_HEADS, HEAD_DIM),
                           q_idx.reshape(bsz, s, N_IDX_HEADS, IDX_DIM), k_idx, w_idx)

    merged = jnp.concatenate([rmsnorm(y_lru, g_grp_lru), rmsnorm(y_attn, g_grp_attn)], axis=-1)
    mix = merged @ w_out
    x = x + gt1 * rmsnorm(mix, g_post_mix)

    h = rmsnorm(x, g_pre_ffn) * (1.0 + sc2) + sh2
    u = causal_dwconv(h @ w_ffn_in, conv_ffn_w, conv_ffn_b)
    u_gate, u_up = jnp.split(u, 2, axis=-1)
    f = (jax.nn.gelu(u_gate) * u_up) @ w_ffn_out
    return x + gt2 * rmsnorm(f, g_post_ffn)


def setup_inputs(seed: int = 0) -> dict:
    key = jax.random.key(seed)
    ks = jax.random.split(key, 24)
    f32 = jnp.float32
    nrm = lambda k, shape, s: jax.random.normal(k, shape, f32) * s
    L = DEPTH
    u = jax.random.uniform(ks[15], (L, D_LRU), f32, 0.9, 0.999)
    a0 = u ** (1.0 / LRU_C)
    lru_lambda = jnp.log(a0) - jnp.log1p(-a0)
    return {
        'x': nrm(ks[0], (BATCH, SEQ, D_MODEL), 1.0),
        'c': nrm(ks[1], (BATCH, D_MODEL), 1.0),
        'w_ada': nrm(ks[2], (L, D_MODEL, N_MOD * D_MODEL), 0.5 * D_MODEL ** -0.5),
        'b_ada': nrm(ks[3], (L, N_MOD * D_MODEL), 0.02),
        'g_pre_mix': 1.0 + nrm(ks[4], (L, D_MODEL), 0.02),
        'g_post_mix': 1.0 + nrm(ks[5], (L, D_MODEL), 0.02),
        'g_pre_ffn': 1.0 + nrm(ks[6], (L, D_MODEL), 0.02),
        'g_post_ffn': 1.0 + nrm(ks[7], (L, D_MODEL), 0.02),
        'w_in': nrm(ks[8], (L, D_MODEL, D_IN), D_MODEL ** -0.5),
        'conv_lru_w': nrm(ks[9], (L, CONV_LRU, D_LRU), CONV_LRU ** -0.5),
        'conv_lru_b': nrm(ks[10], (L, D_LRU), 0.02),
        'w_rg_a': nrm(ks[11], (L, N_LRU_BLOCKS, LRU_BLOCK, LRU_BLOCK), LRU_BLOCK ** -0.5),
        'b_rg_a': nrm(ks[12], (L, D_LRU), 0.02),
        'w_rg_x': nrm(ks[13], (L, N_LRU_BLOCKS, LRU_BLOCK, LRU_BLOCK), LRU_BLOCK ** -0.5),
        'b_rg_x': nrm(ks[14], (L, D_LRU), 0.02),
        'lru_lambda': lru_lambda,
        'g_grp_lru': 1.0 + nrm(ks[16], (L, D_LRU), 0.02),
        'g_grp_attn': 1.0 + nrm(ks[17], (L, D_ATTN), 0.02),
        'w_out': nrm(ks[18], (L, D_MIX, D_MODEL), D_MIX ** -0.5),
        'w_ffn_in': nrm(ks[19], (L, D_MODEL, 2 * D_FF), D_MODEL ** -0.5),
        'conv_ffn_w': nrm(ks[20], (L, CONV_FFN, 2 * D_FF), CONV_FFN ** -0.5),
        'conv_ffn_b': nrm(ks[21], (L, 2 * D_FF), 0.02),
        'w_ffn_out': nrm(ks[22], (L, D_FF, D_MODEL), D_FF ** -0.5),
    }


def reference(x, c, w_ada, b_ada, g_pre_mix, g_post_mix, g_pre_ffn, g_post_ffn, w_in,
              conv_lru_w, conv_lru_b, w_rg_a, b_rg_a, w_rg_x, b_rg_x, lru_lambda,
              g_grp_lru, g_grp_attn, w_out, w_ffn_in, conv_ffn_w, conv_ffn_b, w_ffn_out):
    for l in range(DEPTH):
        x = hybrid_layer(x, c, w_ada[l], b_ada[l], g_pre_mix[l], g_post_mix[l], g_pre_ffn[l],
                         g_post_ffn[l], w_in[l], conv_lru_w[l], conv_lru_b[l], w_rg_a[l], b_rg_a[l],
                         w_rg_x[l], b_rg_x[l], lru_lambda[l], g_grp_lru[l], g_grp_attn[l], w_out[l],
                         w_ffn_in[l], conv_ffn_w[l], conv_ffn_b[l], w_ffn_out[l])
    return x
```

```python
import numpy as np
from contextlib import ExitStack
import concourse.bass as bass
import concourse.mybir as mybir
from concourse.bass_utils import run_bass_kernel_spmd

F32 = mybir.dt.float32
BF16 = mybir.dt.bfloat16
AF = mybir.ActivationFunctionType
ALU = mybir.AluOpType
AX = mybir.AxisListType

D = 4096
KC = 32
W = 4096
HALO0 = 1920
OWN0 = 2048
NOWN = W - HALO0
NQT = NOWN // 128
D_LRU = 2048
D_FF = 11008
NFC = D_FF // 128
D_IN = 9312
OFF_GATE, OFF_Q, OFF_K, OFF_V, OFF_QI, OFF_KI, OFF_WI = 2048, 4096, 6144, 6656, 7168, 9216, 9280
EPS = 1e-6
NEG = -1.0e30
TOPK = 256
NBIS = 16

VEC = {}
_off = 0
for _n, _c in [("g_pre_mix", 32), ("g_post_mix", 32), ("g_pre_ffn", 32), ("g_post_ffn", 32), ("b_ada", 192),
               ("conv_lru_w", 64), ("conv_lru_b", 16), ("b_rg_a", 16), ("b_rg_x", 16), ("lru_lambda", 16),
               ("g_grp_lru", 16), ("g_grp_attn", 16), ("conv_ffn_w", 516), ("conv_ffn_b", 172), ("c", 32),
               ("flag", 1)]:
    VEC[_n] = (_off, _c)
    _off += _c
NVEC = _off


class Res:
    __slots__ = ("name", "w", "r", "dsem", "dcount")

    def __init__(self, name):
        self.name = name
        self.w = {}
        self.r = {}
        self.dsem = None
        self.dcount = 0


class Trk:
    def __init__(self, nc):
        self.nc = nc
        self.eng = {"pe": nc.tensor, "act": nc.scalar, "dve": nc.vector, "pool": nc.gpsimd, "sp": nc.sync}
        self.sem = {}
        self.cnt = {}
        self.waited = {k: {} for k in self.eng}
        self.nsem = 0
        self.ninst = {k: 0 for k in self.eng}
        self.dslots = []
        self.dmax = {}
        self.free_d = []

    def release(self, res):
        if res.dsem is not None:
            self.free_d.append((res.dsem, res.dcount))
            res.dsem = None

    def barrier(self):
        deps = [(sem, self.cnt[e], e) for e, sem in self.sem.items()]
        deps += [(sem, cnt, "dma") for (sem, cnt) in self.dmax.values()]
        for e in self.eng:
            self._wait(e, [d for d in deps if d[2] != e or e != "pe"])

    def _newsem(self, tag):
        self.nsem += 1
        return self.nc.alloc_semaphore(f"{tag}{self.nsem}")

    def _wait(self, e, deps):
        best = {}
        for (sem, val, src) in deps:
            if src == "pe" and e == "pe":
                continue
            k = id(sem)
            if k not in best or best[k][1] < val:
                best[k] = (sem, val)
        wd = self.waited[e]
        for k, (sem, val) in best.items():
            if wd.get(k, 0) >= val:
                continue
            self.eng[e].wait_ge(sem, val)
            wd[k] = val

    @staticmethod
    def _deps(reads, writes, bulk=()):
        deps = []
        for r in reads:
            deps.extend(r.w.values())
        for w in writes:
            deps.extend(w.w.values())
            deps.extend(w.r.values())
        for b in bulk:
            deps.extend(b.r.values())
        return deps

    def _signal(self, e, inst, reads, writes):
        if e not in self.sem or self.cnt[e] >= 30000:
            self.sem[e] = self._newsem(e)
            self.cnt[e] = 0
        self.cnt[e] += 1
        inst.then_inc(self.sem[e], 1)
        ev = (self.sem[e], self.cnt[e], e)
        k = id(self.sem[e])
        for r in reads:
            r.r[k] = ev
        for w in writes:
            w.w = {k: ev}
            w.r = {}

    def op(self, e, fn, reads=(), writes=()):
        self._wait(e, self._deps(reads, writes))
        inst = fn(self.eng[e])
        self.ninst[e] += 1
        self._signal(e, inst, reads, writes)
        return inst

    def group(self, e, fns, reads=(), writes=()):
        self._wait(e, self._deps(reads, writes))
        inst = None
        for fn in fns:
            inst = fn(self.eng[e])
            self.ninst[e] += 1
        self._signal(e, inst, reads, writes)
        return inst

    def dma(self, q, out, in_, slot, reads=(), writes=(), bulk=()):
        self._wait(q, self._deps(reads, writes, bulk))
        if slot.dsem is None:
            if self.free_d:
                slot.dsem, slot.dcount = self.free_d.pop()
            else:
                slot.dsem = self._newsem("d")
        inst = self.eng[q].dma_start(out=out, in_=in_)
        self.ninst[q] += 1
        slot.dcount += 16
        inst.then_inc(slot.dsem, 16)
        self.dmax[id(slot.dsem)] = (slot.dsem, slot.dcount)
        ev = (slot.dsem, slot.dcount, "dma")
        k = id(slot.dsem)
        for r in reads:
            r.r[k] = ev
        for w in writes:
            w.w = {k: ev}
            w.r = {}
        for b in bulk:
            b.w[k] = ev
        return inst

    def final_wait(self, e, res):
        self._wait(e, self._deps([res], []))


_UID = [0]
_TRK = [None]


def _u(name):
    _UID[0] += 1
    return f"{name}_{_UID[0]}"


class Slots:
    def __init__(self, es, nc, name, shape, dtype, n):
        self.items = []
        for i in range(n):
            t = es.enter_context(nc.sbuf_tensor(_u(f"{name}{i}"), shape, dtype))
            rres = Res(f"{name}{i}")
            es.callback(_TRK[0].release, rres)
            self.items.append((t, rres))
        self.i = 0

    def next(self):
        it = self.items[self.i % len(self.items)]
        self.i += 1
        return it


class PSlots:
    def __init__(self, es, nc, name, shape, dtype, n):
        self.items = []
        for i in range(n):
            t = es.enter_context(nc.psum_tensor(_u(f"{name}{i}"), shape, dtype))
            self.items.append((t, Res(f"{name}{i}")))
        self.i = 0

    def next(self):
        it = self.items[self.i % len(self.items)]
        self.i += 1
        return it


def build(stop_after=99, dbg=False):
    nc = bass.Bass("TRN2", target_bir_lowering=False)
    T = Trk(nc)
    _TRK[0] = T

    def din(name, shape, dt=F32):
        return nc.dram_tensor(name, shape, dt, kind="ExternalInput").ap()

    def dscr(name, shape, dt=F32):
        kind = "ExternalOutput" if (dbg and name in dbg) else "Internal"
        return nc.dram_tensor(name, shape, dt, kind=kind).ap()

    xw = din("xw", [W, D])
    vecs_d = din("vecs", [128, NVEC])
    kbias_d = din("kbias", [128, W])
    ident_d = din("ident", [128, 128])
    tri_d = din("tri", [128, 128])
    w_ada = din("w_ada", [D, 6 * D])
    w_in = din("w_in", [D, D_IN])
    w_rg_a = din("w_rg_a", [16, 128, 128])
    w_rg_x = din("w_rg_x", [16, 128, 128])
    w_out = din("w_out", [D, D])
    w_f1 = din("w_ffn_in", [D, 2 * D_FF])
    w_f2 = din("w_ffn_out", [D_FF, D])
    out_d = nc.dram_tensor("out", [2048, D], F32, kind="ExternalOutput").ap()

    xl_scr = dscr("xl_scr", [D_LRU, W])
    gl_scr = dscr("gl_scr", [D_LRU, NOWN])
    q_scr = dscr("q_scr", [2048, NOWN], BF16)
    qi_scr = dscr("qi_scr", [2048, NOWN], BF16)
    kt_scr = dscr("kt_scr", [512, W], BF16)
    v_scr = dscr("v_scr", [W, 512], BF16)
    ki_scr = dscr("ki_scr", [128, W], BF16)
    xT_scr = dscr("xT_scr", [D, NOWN])
    mg_scr = dscr("mg_scr", [D, NOWN], BF16)
    x1T_scr = dscr("x1T_scr", [D, NOWN])
    fT_scr = dscr("fT_scr", [D, 512])
    R_xl, R_gl, R_q, R_qi, R_kt, R_v, R_ki, R_xT, R_mg, R_x1T, R_fT, R_out = [
        Res(n) for n in ("xl", "gl", "q", "qi", "kt", "v", "ki", "xT", "mg", "x1T", "fT", "out")]
    R_in = Res("inputs")

    es0 = ExitStack()
    with es0:
        sb = lambda name, shape, dt=F32, es=es0: es.enter_context(nc.sbuf_tensor(_u(name), shape, dt))
        vecs = sb("vecs_sb", [128, NVEC]); R_vecs = Res("vecs")
        ident = sb("ident_sb", [128, 128]); R_ident = Res("ident")
        identb = sb("identb", [128, 128], BF16); R_identb = Res("identb")
        onesb = sb("onesb", [128, 128], BF16); R_onesb = Res("onesb")
        one1 = sb("one1", [1, 1]); R_one1 = Res("one1")
        mod = sb("mod", [128, 192]); R_mod = Res("mod")
        A1 = sb("A1", [128, 32]); G1 = sb("G1", [128, 32]); A2 = sb("A2", [128, 32]); G2 = sb("G2", [128, 32])
        R_der = Res("derived")
        cneg = sb("cneg", [128, 16]); cneg2 = sb("cneg2", [128, 16])
        WI = sb("WI", [128, NQT, 32]); R_WI = Res("WI")

        def V(name, j=0, n=1):
            o, c = VEC[name]
            return vecs[:, o + j:o + j + n]

        T.dma("sp", vecs[:, :], vecs_d[:, :], R_vecs, reads=[R_in], writes=[R_vecs])
        T.dma("sp", ident[:, :], ident_d[:, :], R_ident, reads=[R_in], writes=[R_ident])
        T.op("dve", lambda e: e.tensor_copy(out=identb[:, :], in_=ident[:, :]), [R_ident], [R_identb])
        T.op("dve", lambda e: e.memset(onesb[:, :], 1.0), [], [R_onesb])
        T.op("dve", lambda e: e.memset(one1[:, :], 1.0), [], [R_one1])
        SHIFT1, SCALE1, GATE1, SHIFT2, SCALE2, GATE2 = [mod[:, 32 * i:32 * (i + 1)] for i in range(6)]

        with ExitStack() as es:
            silu = sb("silu", [128, 32], BF16, es); R_silu = Res("silu")
            wsl = Slots(es, nc, "wada", [128, KC, 512], BF16, 2)
            rowsb = Slots(es, nc, "rowsb", [1, 512], F32, 2)
            prow = PSlots(es, nc, "prow", [1, 512], F32, 2)
            pmod = es.enter_context(nc.psum_tensor(_u("pmod"), [128, 192], F32)); R_pmod = Res("pmod")
            T.op("act", lambda e: e.activation(out=silu[:, :], in_=V("c", 0, 32), func=AF.Silu), [R_vecs], [R_silu])
            wsrc = w_ada.rearrange("(kc p) c -> p kc c", p=128)
            for jc in range(48):
                wt, rw = wsl.next()
                T.dma("pool", wt[:, :, :], wsrc[:, :, jc * 512:(jc + 1) * 512], rw, reads=[R_in], writes=[rw])
                pr, rpr = prow.next()
                T.group("pe", [
                    (lambda e, kc=kc: e.matmul(pr[:, :], lhsT=silu[:, kc:kc + 1], rhs=wt[:, kc, :],
                                               start=(kc == 0), stop=(kc == KC - 1)))
                    for kc in range(KC)], [R_silu, rw], [rpr])
                rs, rrs = rowsb.next()
                T.op("act", lambda e: e.activation(out=rs[:, :], in_=pr[:, :], func=AF.Copy), [rpr], [rrs])
                T.group("pe", [
                    (lambda e, j=j: e.matmul(pmod[:, jc * 4 + j:jc * 4 + j + 1], lhsT=rs[0:1, j * 128:(j + 1) * 128],
                                             rhs=one1[0:1, 0:1], start=True, stop=True))
                    for j in range(4)], [rrs, R_one1], [R_pmod])
            T.op("dve", lambda e: e.tensor_tensor(out=mod[:, :], in0=pmod[:, :], in1=V("b_ada", 0, 192), op=ALU.add),
                 [R_pmod, R_vecs], [R_mod])
            tmp = sb("tmpder", [128, 32], F32, es); R_tmp = Res("tmpder")
            for (dst, sc, g) in ((A1, SCALE1, "g_pre_mix"), (A2, SCALE2, "g_pre_ffn")):
                T.op("dve", lambda e: e.tensor_scalar(out=tmp[:, :], in0=sc, scalar1=1.0, scalar2=None, op0=ALU.add),
                     [R_mod], [R_tmp])
                T.op("dve", lambda e: e.tensor_tensor(out=dst[:, :], in0=tmp[:, :], in1=V(g, 0, 32), op=ALU.mult),
                     [R_tmp, R_vecs, R_der], [R_der])
            for (dst, gt, g) in ((G1, GATE1, "g_post_mix"), (G2, GATE2, "g_post_ffn")):
                T.op("dve", lambda e: e.tensor_tensor(out=dst[:, :], in0=gt, in1=V(g, 0, 32), op=ALU.mult),
                     [R_mod, R_vecs, R_der], [R_der])
            t16 = sb("t16", [128, 16], F32, es); R_t16 = Res("t16")
            T.op("act", lambda e: e.activation(out=t16[:, :], in_=V("lru_lambda", 0, 16), func=AF.Exp, scale=-1.0),
                 [R_vecs], [R_t16])
            T.op("act", lambda e: e.activation(out=t16[:, :], in_=t16[:, :], func=AF.Ln, bias=1.0), [R_t16], [R_t16])
            T.op("dve", lambda e: e.tensor_scalar(out=cneg[:, :], in0=t16[:, :], scalar1=-8.0, scalar2=None,
                                                  op0=ALU.mult), [R_t16, R_der], [R_der])
            T.op("dve", lambda e: e.tensor_scalar(out=cneg2[:, :], in0=t16[:, :], scalar1=-16.0, scalar2=None,
                                                  op0=ALU.mult), [R_t16, R_der], [R_der])

        if stop_after >= 1:
            with ExitStack() as es:
                T.barrier()
                hT = sb("hT", [128, KC, 768], BF16, es)
                R_hT = [Res(f"hT{b}") for b in range(6)]
                wsl = Slots(es, nc, "win", [128, KC, 512], BF16, 2)
                xs = Slots(es, nc, "xs", [128, D], F32, 2)
                xn = Slots(es, nc, "xn", [128, D], BF16, 2)
                xTs = Slots(es, nc, "xTs", [128, KC, 128], F32, 1)
                st32 = Slots(es, nc, "st32", [128, 384], F32, 3)
                st16 = Slots(es, nc, "st16", [128, 384], BF16, 3)
                stv = Slots(es, nc, "stv", [128, 512], BF16, 2)
                ssb = Slots(es, nc, "ssb", [128, 2], F32, 2)
                ptr = PSlots(es, nc, "ptr", [128, 8, 128], BF16, 2)
                ptf = PSlots(es, nc, "ptf", [128, 4, 128], F32, 1)
                pmm = PSlots(es, nc, "pmm", [128, 512], F32, 4)
                wsrc = w_in.rearrange("(kc p) c -> p kc c", p=128)
                xT_dst = xT_scr.rearrange("(c p) t -> p c t", p=128)

                tiles = [(0, 768, False), (768, 768, False), (1536, 384, False),
                         (1920, 768, True), (2688, 768, True), (3456, 640, True)]
                for (t0, nt, own) in tiles:
                    nb = nt // 128
                    for b in range(nb):
                        tok = t0 + b * 128
                        x_t, rx = xs.next()
                        T.dma("sp", x_t[:, :], xw[tok:tok + 128, :], rx, reads=[R_in], writes=[rx])
                        xn_t, rxn = xn.next()
                        s_t, rs_ = ssb.next()
                        T.op("act", lambda e: e.activation(out=xn_t[:, :], in_=x_t[:, :], func=AF.Square,
                                                           accum_out=s_t[:, 0:1]), [rx], [rxn, rs_])
                        T.op("act", lambda e: e.activation(out=s_t[:, 1:2], in_=s_t[:, 0:1], func=AF.Ln,
                                                           scale=1.0 / D, bias=EPS), [rs_], [rs_])
                        T.op("act", lambda e: e.activation(out=s_t[:, 1:2], in_=s_t[:, 1:2], func=AF.Exp,
                                                           scale=-0.5), [rs_], [rs_])
                        T.op("dve", lambda e: e.tensor_scalar(out=xn_t[:, :], in0=x_t[:, :], scalar1=s_t[:, 1:2],
                                                              scalar2=None, op0=ALU.mult), [rx, rs_], [rxn])
                        for g8 in range(4):
                            p_t, rp = ptr.next()
                            T.group("pe", [
                                (lambda e, j=j: e.transpose(out=p_t[:, j, :],
                                                            in_=xn_t[:, (g8 * 8 + j) * 128:(g8 * 8 + j + 1) * 128],
                                                            identity=identb[:, :]))
                                for j in range(8)], [rxn, R_identb], [rp])
                            for j in range(8):
                                c = g8 * 8 + j
                                T.op("act", lambda e, c=c, j=j: e.activation(
                                    out=hT[:, c, b * 128:(b + 1) * 128], in_=p_t[:, j, :], func=AF.Identity,
                                    scale=A1[:, c:c + 1], bias=SHIFT1[:, c:c + 1]),
                                    [rp, R_der, R_mod], [R_hT[b]] if c in (0, 31) else [])
                        if own:
                            xt_t, rxt = xTs.next()
                            for g4 in range(8):
                                p_t, rp = ptf.next()
                                T.group("pe", [
                                    (lambda e, j=j: e.transpose(out=p_t[:, j, :],
                                                                in_=x_t[:, (g4 * 4 + j) * 128:(g4 * 4 + j + 1) * 128],
                                                                identity=ident[:, :]))
                                    for j in range(4)], [rx, R_ident], [rp])
                                T.op("dve", lambda e: e.tensor_copy(out=xt_t[:, g4 * 4:(g4 + 1) * 4, :],
                                                                    in_=p_t[:, :, :]), [rp], [rxt])
                            T.dma("sp", xT_dst[:, :, tok - HALO0:tok - HALO0 + 128], xt_t[:, :, :], rxt,
                                  reads=[rxt], bulk=[R_xT])
                    nsub = 2 if nt > 384 else 1
                    nsz = nt // nsub
                    subs = [(i * nsz, nsz) for i in range(nsub)]
                    R_hall = R_hT[:nb]
                    if own:
                        groups = [("xl", i) for i in range(4)] + [("gl", i) for i in range(4)] + \
                                 [("q", i) for i in range(4)] + [("k", 0), ("v", 0)] + \
                                 [("qi", i) for i in range(4)] + [("kw", 0)]
                    else:
                        groups = [("xl", i) for i in range(4)] + [("k", 0), ("v", 0), ("kw", 0)]
                    base = {"xl": 0, "gl": OFF_GATE, "q": OFF_Q, "k": OFF_K, "v": OFF_V, "qi": OFF_QI}
                    for (kind, gi) in groups:
                        wt, rw = wsl.next()
                        if kind == "kw":
                            T.dma("pool", wt[:, :, 0:64], wsrc[:, :, OFF_KI:OFF_KI + 64], rw, reads=[R_in], writes=[rw])
                            T.dma("pool", wt[:, :, 64:128], wsrc[:, :, OFF_KI:OFF_KI + 64], rw, reads=[R_in], bulk=[rw])
                            T.dma("pool", wt[:, :, 128:160], wsrc[:, :, OFF_WI:OFF_WI + 32], rw, reads=[R_in], bulk=[rw])
                        else:
                            c0 = base[kind] + gi * 512
                            T.dma("pool", wt[:, :, :], wsrc[:, :, c0:c0 + 512], rw, reads=[R_in], writes=[rw])
                        if kind == "v":
                            for b in range(nb):
                                p_t, rp = pmm.next()
                                T.group("pe", [
                                    (lambda e, kc=kc: e.matmul(p_t[:, :], lhsT=hT[:, kc, b * 128:(b + 1) * 128],
                                                               rhs=wt[:, kc, :], start=(kc == 0), stop=(kc == KC - 1)))
                                    for kc in range(KC)], [R_hT[b], rw], [rp])
                                s_t, rst = stv.next()
                                T.op("act", lambda e: e.activation(out=s_t[:, :], in_=p_t[:, :], func=AF.Copy), [rp], [rst])
                                T.dma("sp", v_scr[t0 + b * 128:t0 + (b + 1) * 128, :], s_t[:, :], rst, reads=[rst], bulk=[R_v])
                            continue
                        if kind == "kw":
                            for (s0, n) in subs:
                                p_t, rp = pmm.next()
                                T.group("pe", [
                                    (lambda e, kc=kc: e.matmul(p_t[:, 0:n], lhsT=wt[:, kc, 0:128], rhs=hT[:, kc, s0:s0 + n],
                                                               start=(kc == 0), stop=(kc == KC - 1)))
                                    for kc in range(KC)], R_hall + [rw], [rp])
                                s_t, rst = st16.next()
                                T.op("act", lambda e: e.activation(out=s_t[:, 0:n], in_=p_t[:, 0:n], func=AF.Copy), [rp], [rst])
                                T.dma("sp", ki_scr[:, t0 + s0:t0 + s0 + n], s_t[:, 0:n], rst, reads=[rst], bulk=[R_ki])
                            if own:
                                for b in range(nb):
                                    p_t, rp = pmm.next()
                                    T.group("pe", [
                                        (lambda e, kc=kc: e.matmul(p_t[:, 0:32], lhsT=hT[:, kc, b * 128:(b + 1) * 128],
                                                                   rhs=wt[:, kc, 128:160], start=(kc == 0), stop=(kc == KC - 1)))
                                        for kc in range(KC)], [R_hT[b], rw], [rp])
                                    qt = (t0 + b * 128 - HALO0) // 128
                                    T.op("act", lambda e: e.activation(out=WI[:, qt, :], in_=p_t[:, 0:32], func=AF.Copy),
                                         [rp], [R_WI])
                            continue
                        for j in range(4):
                            ch = gi * 4 + j
                            for (s0, n) in subs:
                                p_t, rp = pmm.next()
                                T.group("pe", [
                                    (lambda e, kc=kc: e.matmul(p_t[:, 0:n], lhsT=wt[:, kc, j * 128:(j + 1) * 128],
                                                               rhs=hT[:, kc, s0:s0 + n], start=(kc == 0), stop=(kc == KC - 1)))
                                    for kc in range(KC)], R_hall + [rw], [rp])
                                tw = t0 + s0
                                to = tw - HALO0
                                if kind == "xl":
                                    s_t, rst = st32.next()
                                    T.op("act", lambda e: e.activation(out=s_t[:, 0:n], in_=p_t[:, 0:n], func=AF.Copy), [rp], [rst])
                                    T.dma("sp", xl_scr[ch * 128:(ch + 1) * 128, tw:tw + n], s_t[:, 0:n], rst, reads=[rst], bulk=[R_xl])
                                elif kind == "gl":
                                    s_t, rst = st32.next()
                                    T.op("act", lambda e: e.activation(out=s_t[:, 0:n], in_=p_t[:, 0:n], func=AF.Gelu), [rp], [rst])
                                    T.dma("sp", gl_scr[ch * 128:(ch + 1) * 128, to:to + n], s_t[:, 0:n], rst, reads=[rst], bulk=[R_gl])
                                elif kind == "q":
                                    s_t, rst = st16.next()
                                    T.op("act", lambda e: e.activation(out=s_t[:, 0:n], in_=p_t[:, 0:n], func=AF.Copy,
                                                                       scale=float(128 ** -0.5)), [rp], [rst])
                                    T.dma("sp", q_scr[ch * 128:(ch + 1) * 128, to:to + n], s_t[:, 0:n], rst, reads=[rst], bulk=[R_q])
                                elif kind == "qi":
                                    s_t, rst = st16.next()
                                    T.op("act", lambda e: e.activation(out=s_t[:, 0:n], in_=p_t[:, 0:n], func=AF.Copy), [rp], [rst])
                                    T.dma("sp", qi_scr[ch * 128:(ch + 1) * 128, to:to + n], s_t[:, 0:n], rst, reads=[rst], bulk=[R_qi])
                                elif kind == "k":
                                    s_t, rst = st16.next()
                                    T.op("act", lambda e: e.activation(out=s_t[:, 0:n], in_=p_t[:, 0:n], func=AF.Copy), [rp], [rst])
                                    T.dma("sp", kt_scr[j * 128:(j + 1) * 128, tw:tw + n], s_t[:, 0:n], rst, reads=[rst], bulk=[R_kt])

        if stop_after >= 2:
            with ExitStack() as es:
                T.barrier()
                WA = sb("WA", [128, 16, 128], BF16, es); WX = sb("WX", [128, 16, 128], BF16, es)
                R_WA = Res("WA"); R_WX = Res("WX")
                T.dma("pool", WA[:, :, :], w_rg_a.rearrange("n i j -> i n j"), R_WA, reads=[R_in], writes=[R_WA])
                T.dma("pool", WX[:, :, :], w_rg_x.rearrange("n i j -> i n j"), R_WX, reads=[R_in], writes=[R_WX])
                xlb = sb("xlb", [128, 3 + W], F32, es); R_xlb = Res("xlb")
                carry = sb("carry", [128, 16], F32, es); R_carry = Res("carry")
                ssL = sb("ssL", [128, NOWN], F32, es); R_ssL = Res("ssL")
                YL = sb("YL", [128, 16, NOWN], BF16, es); R_YL = Res("YL")
                BL = 1024
                xc = Slots(es, nc, "xc", [128, BL], F32, 2)
                xcb = Slots(es, nc, "xcb", [128, BL], BF16, 2)
                rr = Slots(es, nc, "rr", [128, BL], F32, 2)
                ii = Slots(es, nc, "ii", [128, BL], F32, 2)
                aa = Slots(es, nc, "aa", [128, BL], F32, 2)
                s2 = Slots(es, nc, "s2", [128, BL], F32, 2)
                hb = Slots(es, nc, "hb", [128, BL], F32, 2)
                glt = Slots(es, nc, "glt", [128, BL], F32, 2)
                yy = Slots(es, nc, "yy", [128, BL], F32, 1)
                ysq = Slots(es, nc, "ysq", [128, BL], BF16, 1)
                pg = PSlots(es, nc, "pg", [128, 512], F32, 4)
                pss = PSlots(es, nc, "pss", [128, 512], F32, 2)
                T.op("dve", lambda e: e.memset(xlb[:, 0:3], 0.0), [], [R_xlb])
                T.op("dve", lambda e: e.memset(ssL[:, :], 0.0), [], [R_ssL])
                cw = lambda j, ch: V("conv_lru_w", j * 16 + ch)
                xlb2 = sb("xlb2", [128, 3 + W], F32, es); R_xlb2 = Res("xlb2")
                T.op("dve", lambda e: e.memset(xlb2[:, 0:3], 0.0), [], [R_xlb2])
                xlbs = [(xlb, R_xlb), (xlb2, R_xlb2)]
                for pr_ in range(8):
                    for k_ in range(2):
                        ch = 2 * pr_ + k_; xlb, R_xlb = xlbs[k_]
                        T.dma("sp", xlb[:, 3:3 + W], xl_scr[ch * 128:(ch + 1) * 128, :], R_xlb, reads=[R_xl], writes=[R_xlb])
                        T.op("dve", lambda e: e.tensor_scalar(out=xlb[:, OWN0:OWN0 + 3], in0=xlb[:, OWN0:OWN0 + 3],
                                                              scalar1=V("flag"), scalar2=None, op0=ALU.mult),
                             [R_vecs], [R_xlb])
                    for b in range(W // BL):
                        for k_ in range(2):
                            ch = 2 * pr_ + k_; xlb, R_xlb = xlbs[k_]
                            c0 = b * BL
                            xc_t, rxc = xc.next()
                            T.op("act", lambda e: e.activation(out=xc_t[:, :], in_=xlb[:, 3 + c0:3 + c0 + BL], func=AF.Identity,
                                                               scale=cw(3, ch), bias=V("conv_lru_b", ch)), [R_xlb, R_vecs], [rxc])
                            for j in (2, 1, 0):
                                T.op("dve", lambda e, j=j: e.scalar_tensor_tensor(
                                    out=xc_t[:, :], in0=xlb[:, j + c0:j + c0 + BL], scalar=cw(j, ch), in1=xc_t[:, :],
                                    op0=ALU.mult, op1=ALU.add), [R_xlb, R_vecs], [rxc])
                            xb_t, rxb = xcb.next()
                            T.op("act", lambda e: e.activation(out=xb_t[:, :], in_=xc_t[:, :], func=AF.Copy), [rxc], [rxb])
                            r_t, rr_ = rr.next(); i_t, ri_ = ii.next()
                            for sub in range(BL // 512):
                                sl = slice(sub * 512, (sub + 1) * 512)
                                pa, rpa = pg.next()
                                T.group("pe", [lambda e: e.matmul(pa[:, :], lhsT=WA[:, ch, :], rhs=xb_t[:, sl], start=True, stop=True)],
                                        [R_WA, rxb], [rpa])
                                T.op("act", lambda e: e.activation(out=r_t[:, sl], in_=pa[:, :], func=AF.Sigmoid,
                                                                   bias=V("b_rg_a", ch)), [rpa, R_vecs], [rr_])
                                px, rpx = pg.next()
                                T.group("pe", [lambda e: e.matmul(px[:, :], lhsT=WX[:, ch, :], rhs=xb_t[:, sl], start=True, stop=True)],
                                        [R_WX, rxb], [rpx])
                                T.op("act", lambda e: e.activation(out=i_t[:, sl], in_=px[:, :], func=AF.Sigmoid,
                                                                   bias=V("b_rg_x", ch)), [rpx, R_vecs], [ri_])
                            a_t, ra_ = aa.next(); s_t, rs_ = s2.next()
                            T.op("act", lambda e: e.activation(out=a_t[:, :], in_=r_t[:, :], func=AF.Exp, scale=cneg[:, ch:ch + 1]),
                                 [rr_, R_der], [ra_])
                            T.op("act", lambda e: e.activation(out=s_t[:, :], in_=r_t[:, :], func=AF.Exp, scale=cneg2[:, ch:ch + 1]),
                                 [rr_, R_der], [rs_])
                            T.op("act", lambda e: e.activation(out=s_t[:, :], in_=s_t[:, :], func=AF.Sqrt, scale=-1.0, bias=1.0),
                                 [rs_], [rs_])
                            T.op("dve", lambda e: e.tensor_tensor(out=i_t[:, :], in0=i_t[:, :], in1=xc_t[:, :], op=ALU.mult),
                                 [ri_, rxc], [ri_])
                            T.op("dve", lambda e: e.tensor_tensor(out=i_t[:, :], in0=i_t[:, :], in1=s_t[:, :], op=ALU.mult),
                                 [ri_, rs_], [ri_])
                            h_t, rh = hb.next()
                            init = 0.0 if b == 0 else carry[:, ch:ch + 1]
                            T.op("dve", lambda e: e.tensor_tensor_scan(out=h_t[:, :], data0=a_t[:, :], data1=i_t[:, :], initial=init,
                                                                       op0=ALU.mult, op1=ALU.add), [ra_, ri_, R_carry], [rh])
                            if c0 + BL == OWN0:
                                T.op("dve", lambda e: e.tensor_scalar(out=carry[:, ch:ch + 1], in0=h_t[:, BL - 1:BL], scalar1=V("flag"),
                                                                      scalar2=None, op0=ALU.mult), [rh, R_vecs], [R_carry])
                            else:
                                T.op("dve", lambda e: e.tensor_copy(out=carry[:, ch:ch + 1], in_=h_t[:, BL - 1:BL]), [rh], [R_carry])
                            lo = max(c0, HALO0)
                            if lo < c0 + BL:
                                n = c0 + BL - lo; off = lo - c0; orel = lo - HALO0
                                g_t, rg = glt.next()
                                T.dma("sp", g_t[:, 0:n], gl_scr[ch * 128:(ch + 1) * 128, orel:orel + n], rg, reads=[R_gl], writes=[rg])
                                y_t, ry = yy.next()
                                T.op("dve", lambda e: e.tensor_tensor(out=y_t[:, 0:n], in0=h_t[:, off:off + n], in1=g_t[:, 0:n], op=ALU.mult),
                                     [rh, rg], [ry])
                                T.op("act", lambda e: e.activation(out=YL[:, ch, orel:orel + n], in_=y_t[:, 0:n], func=AF.Copy), [ry], [R_YL])
                                q_t, rq = ysq.next()
                                T.op("act", lambda e: e.activation(out=q_t[:, 0:n], in_=y_t[:, 0:n], func=AF.Square), [ry], [rq])
                                for p0 in range(0, n, 512):
                                    pn = min(512, n - p0)
                                    ps_t, rps = pss.next()
                                    T.group("pe", [lambda e: e.matmul(ps_t[:, 0:pn], lhsT=onesb[:, :], rhs=q_t[:, p0:p0 + pn], start=True, stop=True)],
                                            [R_onesb, rq], [rps])
                                    T.op("dve", lambda e: e.tensor_tensor(out=ssL[:, orel + p0:orel + p0 + pn], in0=ps_t[:, 0:pn],
                                                                          in1=ssL[:, orel + p0:orel + p0 + pn], op=ALU.add), [rps], [R_ssL])
                T.op("act", lambda e: e.activation(out=ssL[:, :], in_=ssL[:, :], func=AF.Ln, scale=1.0 / D_LRU, bias=EPS), [R_ssL], [R_ssL])
                T.op("act", lambda e: e.activation(out=ssL[:, :], in_=ssL[:, :], func=AF.Exp, scale=-0.5), [R_ssL], [R_ssL])
                mgs = Slots(es, nc, "mgs", [128, NOWN], BF16, 2)
                for ch in range(16):
                    m_t, rm = mgs.next()
                    T.op("dve", lambda e: e.scalar_tensor_tensor(out=m_t[:, :], in0=YL[:, ch, :], scalar=V("g_grp_lru", ch), in1=ssL[:, :],
                                                                 op0=ALU.mult, op1=ALU.mult), [R_YL, R_ssL, R_vecs], [rm])
                    T.dma("sp", mg_scr[ch * 128:(ch + 1) * 128, :], m_t[:, :], rm, reads=[rm], bulk=[R_mg])

        if stop_after >= 3:
            with ExitStack() as es:
                T.barrier()
                KT = sb("KT", [128, 4, W], BF16, es); R_KT = Res("KT")
                Vv = sb("Vv", [128, 32, 512], BF16, es); R_Vv = Res("Vv")
                KI = sb("KI", [128, W], BF16, es); R_KI = Res("KI")
                kb_sb = sb("kb_sb", [128, W], BF16, es); R_kb = Res("kb")
                tri = sb("tri_sb", [128, 128], F32, es); R_tri = Res("tri")
                T.dma("sp", KI[:, :], ki_scr[:, :], R_KI, reads=[R_ki], writes=[R_KI])
                T.dma("pool", kb_sb[:, :], kbias_d[:, :], R_kb, reads=[R_in], writes=[R_kb])
                T.dma("sp", tri[:, :], tri_d[:, :], R_tri, reads=[R_in], writes=[R_tri])
                T.dma("sp", KT[:, :, :], kt_scr.rearrange("(g p) t -> p g t", p=128), R_KT, reads=[R_kt], writes=[R_KT])
                T.dma("sp", Vv[:, :, :], v_scr.rearrange("(b p) c -> p b c", p=128), R_Vv, reads=[R_v], writes=[R_Vv])
                for rr_ in (R_KT, R_Vv, R_KI, R_kb, R_tri):
                    es.callback(T.release, rr_)
                qiT = Slots(es, nc, "qiT", [128, 16, 128], BF16, 2)
                qT = Slots(es, nc, "qT", [128, 16, 128], BF16, 2)
                score = Slots(es, nc, "score", [128, W], F32, 2)
                Mt = Slots(es, nc, "Mt", [128, W], BF16, 2)
                MTs = Slots(es, nc, "MTs", [128, 32, 128], BF16, 2)
                rl = Slots(es, nc, "rl", [128, 512], F32, 4)
                Pt = Slots(es, nc, "Pt", [128, 4, 128], BF16, 4)
                stt = Slots(es, nc, "stt", [128, 8], F32, 3)
                rinv = Slots(es, nc, "rinv", [128, 512], F32, 1)
                YAq = Slots(es, nc, "YAq", [128, 16, 128], F32, 1)
                ysqa = Slots(es, nc, "ysqa", [128, 512], BF16, 2)
                rab = Slots(es, nc, "rab", [128, 128], F32, 2)
                mst = Slots(es, nc, "mst", [128, 16, 128], BF16, 1)
                pd = PSlots(es, nc, "pd", [128, 512], F32, 2)
                pS = PSlots(es, nc, "pS", [128, 512], F32, 2)
                pO = PSlots(es, nc, "pO", [128, 512], F32, 1)
                pR = PSlots(es, nc, "pR", [128, 512], F32, 1)
                ptm = PSlots(es, nc, "ptm", [128, 8, 128], BF16, 1)
                pssa = PSlots(es, nc, "pssa", [128, 128], F32, 1)
                qi_src = qi_scr.rearrange("(c p) t -> p c t", p=128)
                q_src = q_scr.rearrange("(c p) t -> p c t", p=128)
                mg_dst = mg_scr.rearrange("(c p) t -> p c t", p=128)
                state = {}

                def stageA(j):
                    orel = j * 128
                    n = HALO0 + orel + 128
                    qi_t, rqi = qiT.next()
                    T.dma("sp", qi_t[:, :, :], qi_src[:, :, orel:orel + 128], rqi, reads=[R_qi], writes=[rqi])
                    sc_t, rsc = score.next()
                    state[("score", j)] = (sc_t, rsc)
                    items = [(s0, min(512, n - s0), hd) for s0 in range(0, n, 512) for hd in range(32)]
                    NI = len(items)
                    pts = {}
                    rts = {}

                    def mm(t):
                        s0, ns, hd = items[t]
                        c, hh = hd // 2, hd % 2
                        p_t, rp = pd.next()
                        pts[t] = (p_t, rp)
                        T.group("pe", [lambda e: e.matmul(p_t[:, 0:ns], lhsT=qi_t[hh * 64:(hh + 1) * 64, c, :],
                                                          rhs=KI[hh * 64:(hh + 1) * 64, s0:s0 + ns], start=True, stop=True)],
                                [rqi, R_KI], [rp])

                    def relu(t):
                        s0, ns, hd = items[t]
                        p_t, rp = pts.pop(t)
                        r_t, rr_ = rl.next()
                        rts[t] = (r_t, rr_)
                        T.op("act", lambda e: e.activation(out=r_t[:, 0:ns], in_=p_t[:, 0:ns], func=AF.Relu), [rp], [rr_])

                    def acc(t):
                        s0, ns, hd = items[t]
                        r_t, rr_ = rts.pop(t)
                        if hd == 0:
                            T.op("dve", lambda e: e.tensor_scalar(out=sc_t[:, s0:s0 + ns], in0=r_t[:, 0:ns], scalar1=WI[:, j, hd:hd + 1],
                                                                  scalar2=None, op0=ALU.mult), [rr_, R_WI], [rsc])
                        else:
                            T.op("dve", lambda e: e.scalar_tensor_tensor(out=sc_t[:, s0:s0 + ns], in0=r_t[:, 0:ns],
                                                                         scalar=WI[:, j, hd:hd + 1], in1=sc_t[:, s0:s0 + ns],
                                                                         op0=ALU.mult, op1=ALU.add), [rr_, R_WI], [rsc])

                    mm(0)
                    for t in range(NI + 1):
                        if t + 1 < NI:
                            mm(t + 1)
                        if t < NI:
                            relu(t)
                        if t >= 2:
                            acc(t - 2)
                        yield
                    acc(NI - 1)

                def stageB(j):
                    orel = j * 128
                    n = HALO0 + orel + 128
                    nkb = n // 128
                    sc_t, rsc = state.pop(("score", j))
                    st_t, rst = stt.next()
                    LO, HI, MID, CNT, PW, RNG = [st_t[:, k:k + 1] for k in range(6)]
                    T.op("dve", lambda e: e.tensor_reduce(out=LO, in_=sc_t[:, 0:n], axis=AX.X, op=ALU.min), [rsc], [rst])
                    T.op("dve", lambda e: e.tensor_reduce(out=HI, in_=sc_t[:, 0:n], axis=AX.X, op=ALU.max), [rsc], [rst])
                    yield
                    T.op("dve", lambda e: e.tensor_tensor(out=RNG, in0=HI, in1=LO, op=ALU.subtract), [], [rst])
                    T.op("dve", lambda e: e.tensor_tensor(out=sc_t[:, 0:n], in0=sc_t[:, 0:n], in1=kb_sb[:, 0:n], op=ALU.add), [R_kb], [rsc])
                    T.op("dve", lambda e: e.tensor_tensor(out=sc_t[:, n - 128:n], in0=sc_t[:, n - 128:n], in1=tri[:, :], op=ALU.add),
                         [R_tri], [rsc])
                    yield
                    m_t, rm = Mt.next()
                    for it in range(NBIS):
                        wk = float(2.0 ** -(it + 1))
                        T.op("dve", lambda e: e.tensor_scalar(out=MID, in0=RNG, scalar1=wk, scalar2=LO, op0=ALU.mult, op1=ALU.add), [], [rst])
                        T.op("dve", lambda e: e.tensor_scalar(out=m_t[:, 0:n], in0=sc_t[:, 0:n], scalar1=MID, scalar2=0.0,
                                                              op0=ALU.is_ge, op1=ALU.add, accum_out=CNT), [rsc], [rm, rst])
                        T.op("dve", lambda e: e.tensor_scalar(out=PW, in0=CNT, scalar1=float(TOPK), scalar2=wk, op0=ALU.is_ge, op1=ALU.mult),
                             [], [rst])
                        T.op("dve", lambda e: e.scalar_tensor_tensor(out=LO, in0=PW, scalar=RNG, in1=LO, op0=ALU.mult, op1=ALU.add), [], [rst])
                        yield
                    T.op("dve", lambda e: e.tensor_scalar(out=LO, in0=LO, scalar1=-1.0e29, scalar2=None, op0=ALU.max), [], [rst])
                    T.op("dve", lambda e: e.tensor_scalar(out=m_t[:, 0:n], in0=sc_t[:, 0:n], scalar1=LO, scalar2=None, op0=ALU.is_ge),
                         [rsc, rst], [rm])
                    yield
                    mt_t, rmt = MTs.next()
                    state[("mt", j)] = (mt_t, rmt)
                    for k0 in range(0, nkb, 8):
                        m8 = min(8, nkb - k0)
                        p_t, rp = ptm.next()
                        T.group("pe", [(lambda e, i=i: e.transpose(out=p_t[:, i, :], in_=m_t[:, (k0 + i) * 128:(k0 + i + 1) * 128],
                                                                   identity=identb[:, :])) for i in range(m8)], [rm, R_identb], [rp])
                        T.op("act", lambda e: e.activation(out=mt_t[:, k0:k0 + m8, :], in_=p_t[:, 0:m8, :], func=AF.Copy), [rp], [rmt])
                        yield

                def stageC(j):
                    orel = j * 128
                    n = HALO0 + orel + 128
                    nkb = n // 128
                    mt_t, rmt = state.pop(("mt", j))
                    q_t, rq = qT.next()
                    T.dma("sp", q_t[:, :, :], q_src[:, :, orel:orel + 128], rq, reads=[R_q], writes=[rq])
                    ya_t, rya = YAq.next()
                    pss_t, rpss = pssa.next()
                    steps = [(g, kb) for g in range(4) for kb in range(nkb)]
                    NS = len(steps)
                    sts = {}
                    pps = {}
                    accs = {}

                    def smm(t):
                        g, kb = steps[t]
                        s_t, rs_ = pS.next()
                        sts[t] = (s_t, rs_)
                        T.group("pe", [lambda e: e.matmul(s_t[:, :], lhsT=KT[:, g, kb * 128:(kb + 1) * 128],
                                                          rhs=q_t[:, 4 * g:4 * g + 4, :], start=True, stop=True)], [R_KT, rq], [rs_])

                    def expmask(t):
                        g, kb = steps[t]
                        s_t, rs_ = sts.pop(t)
                        p_t, rp = Pt.next()
                        pps[t] = (p_t, rp)
                        T.op("act", lambda e: e.activation(out=p_t[:, :, :], in_=s_t[:, :], func=AF.Exp), [rs_], [rp])
                        T.op("pool", lambda e: e.tensor_tensor(out=p_t[:, :, :], in0=p_t[:, :, :],
                                                               in1=mt_t[:, kb:kb + 1, :].broadcast_to([128, 4, 128]), op=ALU.mult), [rmt], [rp])

                    def pv(t):
                        g, kb = steps[t]
                        p_t, rp = pps.pop(t)
                        if kb == 0:
                            accs[g] = (pO.next(), pR.next())
                        (o_t, ro), (rr_t, rrr) = accs[g]
                        T.group("pe", [
                            lambda e: e.matmul(o_t[:, :], lhsT=Vv[:, kb, g * 128:(g + 1) * 128], rhs=p_t[:, :, :], start=(kb == 0), stop=(kb == nkb - 1)),
                            lambda e: e.matmul(rr_t[:, :], lhsT=onesb[:, :], rhs=p_t[:, :, :], start=(kb == 0), stop=(kb == nkb - 1))],
                            [R_Vv, R_onesb, rp], [ro, rrr])
                        if kb == nkb - 1:
                            ri_t, rri = rinv.next()
                            T.op("act", lambda e: e.activation(out=ri_t[:, :], in_=rr_t[:, :], func=AF.Ln, bias=1.0e-30), [rrr], [rri])
                            T.op("act", lambda e: e.activation(out=ri_t[:, :], in_=ri_t[:, :], func=AF.Exp, scale=-1.0), [], [rri])
                            T.op("dve", lambda e: e.tensor_tensor(out=ya_t[:, 4 * g:4 * g + 4, :], in0=o_t[:, :], in1=ri_t[:, :], op=ALU.mult),
                                 [ro, rri], [rya])
                            yq_t, ryq = ysqa.next()
                            T.op("act", lambda e: e.activation(out=yq_t[:, :], in_=ya_t[:, 4 * g:4 * g + 4, :], func=AF.Square), [rya], [ryq])
                            T.group("pe", [(lambda e, h4=h4: e.matmul(pss_t[:, :], lhsT=onesb[:, :], rhs=yq_t[:, h4 * 128:(h4 + 1) * 128],
                                                                      start=(g == 0 and h4 == 0), stop=(g == 3 and h4 == 3))) for h4 in range(4)],
                                    [R_onesb, ryq], [rpss])

                    smm(0)
                    for t in range(NS + 1):
                        if t + 1 < NS:
                            smm(t + 1)
                        if t < NS:
                            expmask(t)
                        if t >= 2:
                            pv(t - 2)
                        yield
                    pv(NS - 1)
                    ra_t, rra = rab.next()
                    T.op("act", lambda e: e.activation(out=ra_t[:, :], in_=pss_t[:, :], func=AF.Ln, scale=1.0 / 2048, bias=EPS), [rpss], [rra])
                    T.op("act", lambda e: e.activation(out=ra_t[:, :], in_=ra_t[:, :], func=AF.Exp, scale=-0.5), [], [rra])
                    ms_t, rms = mst.next()
                    for hd in range(16):
                        T.op("dve", lambda e: e.scalar_tensor_tensor(out=ms_t[:, hd, :], in0=ya_t[:, hd, :], scalar=V("g_grp_attn", hd),
                                                                     in1=ra_t[:, :], op0=ALU.mult, op1=ALU.mult),
                             [rya, rra, R_vecs], [rms] if hd in (0, 15) else [])
                    T.dma("sp", mg_dst[:, 16:32, orel:orel + 128], ms_t[:, :, :], rms, reads=[rms], bulk=[R_mg])
                    yield

                def nyields(kind, j):
                    n = HALO0 + j * 128 + 128
                    if kind == "A":
                        return 32 * ((n + 511) // 512)
                    if kind == "B":
                        return NBIS + 3 + (n // 128 + 7) // 8
                    return 4 * (n // 128) + 5

                for step in range(NQT + 2):
                    gens = []
                    for kind, fn, jj in (("A", stageA, step), ("B", stageB, step - 1), ("C", stageC, step - 2)):
                        if 0 <= jj < NQT:
                            gens.append([fn(jj), 0, nyields(kind, jj)])
                    while gens:
                        gens.sort(key=lambda g: g[1] / g[2])
                        g = gens[0]
                        try:
                            next(g[0])
                            g[1] += 1
                        except StopIteration:
                            gens.remove(g)

        if stop_after >= 4:
            with ExitStack() as es4:
                T.barrier()
                fcar = sb("fcar", [128, 2 * NFC, 2], F32, es4); R_fcar = Res("fcar")
                T.op("dve", lambda e: e.memset(fcar[:, :, :], 0.0), [], [R_fcar])
                mg_src = mg_scr.rearrange("(c p) t -> p c t", p=128)
                wo_src = w_out.rearrange("(kc p) c -> p kc c", p=128)
                w1_src = w_f1.rearrange("(kc p) c -> p kc c", p=128)
                w2_src = w_f2.rearrange("(c p) o -> p c o", p=128)
                fw = lambda j, cg: V("conv_ffn_w", j * 172 + cg)

                def rstd_from(ps_list, subs, dst, rdst, nfeat):
                    for (p_t, rp), (l0, n) in zip(ps_list, subs):
                        T.op("act", lambda e: e.activation(out=dst[:, l0:l0 + n], in_=p_t[:, 0:n], func=AF.Ln, scale=1.0 / nfeat, bias=EPS),
                             [rp], [rdst])
                    T.op("act", lambda e: e.activation(out=dst[:, :], in_=dst[:, :], func=AF.Exp, scale=-0.5), [], [rdst])

                for r in range(4):
                    if r == 0:
                        o_start, nr, subs = 126, 514, [(0, 2), (2, 512)]
                    else:
                        o_start, nr, subs = 128 + 512 * r, 512, [(0, 512)]
                    ob = 128 + 512 * r
                    with ExitStack() as esr:
                        T.barrier()
                        h2T = sb("h2T", [128, KC, 514], BF16, esr); R_h2T = Res("h2T")
                        rs3 = sb("rs3", [128, 512], F32, esr); R_rs3 = Res("rs3")
                        with ExitStack() as es:
                            mgT = sb("mgT", [128, KC, 514], BF16, es); R_mgT = Res("mgT"); es.callback(T.release, R_mgT)
                            mixT = sb("mixT", [128, KC, 514], F32, es); R_mix = [Res(f"mix{c}") for c in range(KC)]
                            rs1 = sb("rs1", [128, 514], F32, es); R_rs1 = Res("rs1")
                            S_x1 = Res("x1store"); es.callback(T.release, S_x1)
                            rs2 = sb("rs2", [128, 514], F32, es); R_rs2 = Res("rs2")
                            wo = Slots(es, nc, "wo", [128, KC, 256], BF16, 2)
                            sq = Slots(es, nc, "sq", [128, 514], BF16, 3)
                            xTc = Slots(es, nc, "xTc", [128, 514], F32, 4)
                            tmpf = Slots(es, nc, "tmpf", [128, 514], F32, 3)
                            pm = PSlots(es, nc, "pm", [128, 512], F32, 3)
                            pq1 = [PSlots(es, nc, f"pq1{i}", [128, 512], F32, 1).next() for i in range(len(subs))]
                            pq2 = [PSlots(es, nc, f"pq2{i}", [128, 512], F32, 1).next() for i in range(len(subs))]
                            T.dma("sp", mgT[:, :, 0:nr], mg_src[:, :, o_start:o_start + nr], R_mgT, reads=[R_mg], writes=[R_mgT])
                            for og in range(16):
                                wt, rw = wo.next()
                                T.dma("pool", wt[:, :, :], wo_src[:, :, og * 256:(og + 1) * 256], rw, reads=[R_in], writes=[rw])
                                for jj in range(2):
                                    oc = og * 2 + jj
                                    for si, (l0, n) in enumerate(subs):
                                        p_t, rp = pm.next()
                                        T.group("pe", [(lambda e, kc=kc: e.matmul(p_t[:, 0:n], lhsT=wt[:, kc, jj * 128:(jj + 1) * 128],
                                                                                  rhs=mgT[:, kc, l0:l0 + n], start=(kc == 0), stop=(kc == KC - 1)))
                                                       for kc in range(KC)], [rw, R_mgT], [rp])
                                        T.op("act", lambda e: e.activation(out=mixT[:, oc, l0:l0 + n], in_=p_t[:, 0:n], func=AF.Copy),
                                             [rp], [R_mix[oc]])
                                        q_t, rq_ = sq.next()
                                        T.op("act", lambda e: e.activation(out=q_t[:, 0:n], in_=p_t[:, 0:n], func=AF.Square), [rp], [rq_])
                                        T.group("pe", [lambda e: e.matmul(pq1[si][0][:, 0:n], lhsT=onesb[:, :], rhs=q_t[:, 0:n],
                                                                          start=(oc == 0), stop=(oc == KC - 1))], [R_onesb, rq_], [pq1[si][1]])
                            rstd_from(pq1, subs, rs1, R_rs1, D)
                            xq = []

                            def ldx(oc):
                                x_t, rx = xTc.next()
                                T.dma("sp", x_t[:, 0:nr], xT_scr[oc * 128:(oc + 1) * 128, o_start:o_start + nr], rx, reads=[R_xT], writes=[rx])
                                xq.append((x_t, rx))
                            for oc in range(3):
                                ldx(oc)
                            for oc in range(KC):
                                if oc + 3 < KC:
                                    ldx(oc + 3)
                                x_t, rx = xq.pop(0)
                                T.op("dve", lambda e: e.scalar_tensor_tensor(out=mixT[:, oc, 0:nr], in0=mixT[:, oc, 0:nr], scalar=G1[:, oc:oc + 1],
                                                                             in1=rs1[:, 0:nr], op0=ALU.mult, op1=ALU.mult),
                                     [R_rs1, R_der], [R_mix[oc]])
                                T.op("dve", lambda e: e.tensor_tensor(out=mixT[:, oc, 0:nr], in0=mixT[:, oc, 0:nr], in1=x_t[:, 0:nr], op=ALU.add),
                                     [rx], [R_mix[oc]])
                                T.dma("sp", x1T_scr[oc * 128:(oc + 1) * 128, o_start:o_start + nr], mixT[:, oc, 0:nr], S_x1,
                                      reads=[R_mix[oc]], bulk=[R_x1T])
                                for si, (l0, n) in enumerate(subs):
                                    q_t, rq_ = sq.next()
                                    T.op("act", lambda e: e.activation(out=q_t[:, 0:n], in_=mixT[:, oc, l0:l0 + n], func=AF.Square),
                                         [R_mix[oc]], [rq_])
                                    T.group("pe", [lambda e: e.matmul(pq2[si][0][:, 0:n], lhsT=onesb[:, :], rhs=q_t[:, 0:n],
                                                                      start=(oc == 0), stop=(oc == KC - 1))], [R_onesb, rq_], [pq2[si][1]])
                            rstd_from(pq2, subs, rs2, R_rs2, D)
                            for oc in range(KC):
                                t_t, rt = tmpf.next()
                                T.op("dve", lambda e: e.scalar_tensor_tensor(out=t_t[:, 0:nr], in0=mixT[:, oc, 0:nr], scalar=A2[:, oc:oc + 1],
                                                                             in1=rs2[:, 0:nr], op0=ALU.mult, op1=ALU.mult),
                                     [R_mix[oc], R_rs2, R_der], [rt])
                                T.op("act", lambda e: e.activation(out=h2T[:, oc, 0:nr], in_=t_t[:, 0:nr], func=AF.Identity,
                                                                   bias=SHIFT2[:, oc:oc + 1]), [rt, R_mod], [R_h2T] if oc in (0, KC - 1) else [])
                            if r == 0:
                                T.op("dve", lambda e: e.tensor_scalar(out=h2T[:, :, 0:2], in0=h2T[:, :, 0:2], scalar1=V("flag"), scalar2=None,
                                                                      op0=ALU.mult), [R_vecs], [R_h2T])
                        with ExitStack() as esf:
                            T.barrier()
                            actT = sb("actT", [128, NFC, 512], BF16, esf); R_act = Res("actT")
                            hoff = nr - 512
                            with ExitStack() as es:
                                w1 = Slots(es, nc, "w1", [128, KC, 512], BF16, 2)
                                yc = Slots(es, nc, "yc", [128, 514], F32, 2)
                                uu = Slots(es, nc, "uu", [128, 512], F32, 4)
                                py = PSlots(es, nc, "py", [128, 512], F32, 4)
                                for gi in range(NFC // 2):
                                    c0 = gi * 2
                                    wt, rw = w1.next()
                                    T.dma("pool", wt[:, :, 0:256], w1_src[:, :, c0 * 128:c0 * 128 + 256], rw, reads=[R_in], writes=[rw])
                                    T.dma("pool", wt[:, :, 256:512], w1_src[:, :, D_FF + c0 * 128:D_FF + c0 * 128 + 256], rw, reads=[R_in], bulk=[rw])
                                    for pr in range(2):
                                        us = []
                                        for i in (pr, pr + 2):
                                            cg = c0 + i if i < 2 else NFC + c0 + i - 2
                                            u_t, ru = uu.next()
                                            for (l0, n) in subs:
                                                p_t, rp = py.next()
                                                T.group("pe", [(lambda e, kc=kc: e.matmul(p_t[:, 0:n], lhsT=wt[:, kc, i * 128:(i + 1) * 128],
                                                                                          rhs=h2T[:, kc, l0:l0 + n], start=(kc == 0), stop=(kc == KC - 1)))
                                                               for kc in range(KC)], [rw, R_h2T], [rp])
                                                if n == 2:
                                                    T.op("dve", lambda e: e.tensor_copy(out=fcar[:, cg, :], in_=p_t[:, 0:2]), [rp], [R_fcar])
                                                    continue
                                                y_t, ry = yc.next()
                                                T.op("dve", lambda e: e.tensor_copy(out=y_t[:, 0:2], in_=fcar[:, cg, :]), [R_fcar], [ry])
                                                T.op("act", lambda e: e.activation(out=y_t[:, 2:514], in_=p_t[:, 0:512], func=AF.Copy), [rp], [ry])
                                                T.op("act", lambda e: e.activation(out=u_t[:, :], in_=p_t[:, 0:512], func=AF.Identity,
                                                                                   scale=fw(2, cg), bias=V("conv_ffn_b", cg)), [rp, R_vecs], [ru])
                                                T.op("dve", lambda e: e.scalar_tensor_tensor(out=u_t[:, :], in0=y_t[:, 1:513], scalar=fw(1, cg), in1=u_t[:, :],
                                                                                             op0=ALU.mult, op1=ALU.add), [ry, R_vecs], [ru])
                                                T.op("dve", lambda e: e.scalar_tensor_tensor(out=u_t[:, :], in0=y_t[:, 0:512], scalar=fw(0, cg), in1=u_t[:, :],
                                                                                             op0=ALU.mult, op1=ALU.add), [ry, R_vecs], [ru])
                                                T.op("dve", lambda e: e.tensor_copy(out=fcar[:, cg, :], in_=y_t[:, 512:514]), [ry], [R_fcar])
                                            us.append((u_t, ru))
                                        (ug, rug), (uu_, ruu) = us
                                        T.op("act", lambda e: e.activation(out=ug[:, :], in_=ug[:, :], func=AF.Gelu), [], [rug])
                                        T.op("dve", lambda e: e.tensor_tensor(out=actT[:, c0 + pr, :], in0=ug[:, :], in1=uu_[:, :], op=ALU.mult),
                                             [rug, ruu], [R_act])
                            with ExitStack() as es:
                                T.barrier()
                                w2 = Slots(es, nc, "w2", [128, 22, 512], BF16, 3)
                                fst = Slots(es, nc, "fst", [128, 512], F32, 3)
                                sqf = Slots(es, nc, "sqf", [128, 512], BF16, 2)
                                pf = [PSlots(es, nc, f"pf{j}", [128, 512], F32, 1).next() for j in range(4)]
                                pq3 = PSlots(es, nc, "pq3", [128, 512], F32, 1).next()
                                blocks = [(0, 22), (22, 22), (44, 22), (66, 20)]
                                for og in range(8):
                                    for (cb0, nb) in blocks:
                                        wt, rw = w2.next()
                                        T.dma("pool", wt[:, 0:nb, :], w2_src[:, cb0:cb0 + nb, og * 512:(og + 1) * 512], rw, reads=[R_in], writes=[rw])
                                        T.group("pe", [(lambda e, cl=cl, j=j: e.matmul(pf[j][0][:, :], lhsT=wt[:, cl, j * 128:(j + 1) * 128],
                                                                                       rhs=actT[:, cb0 + cl, :], start=(cb0 + cl == 0),
                                                                                       stop=(cb0 + cl == NFC - 1)))
                                                       for cl in range(nb) for j in range(4)], [rw, R_act], [pf[j][1] for j in range(4)])
                                    for j in range(4):
                                        oc = og * 4 + j
                                        f_t, rf = fst.next()
                                        T.op("act", lambda e: e.activation(out=f_t[:, :], in_=pf[j][0][:, :], func=AF.Copy), [pf[j][1]], [rf])
                                        T.dma("sp", fT_scr[oc * 128:(oc + 1) * 128, :], f_t[:, :], rf, reads=[rf], bulk=[R_fT])
                                        q_t, rq_ = sqf.next()
                                        T.op("act", lambda e: e.activation(out=q_t[:, :], in_=pf[j][0][:, :], func=AF.Square), [pf[j][1]], [rq_])
                                        T.group("pe", [lambda e: e.matmul(pq3[0][:, :], lhsT=onesb[:, :], rhs=q_t[:, :], start=(oc == 0), stop=(oc == KC - 1))],
                                                [R_onesb, rq_], [pq3[1]])
                                rstd_from([pq3], [(0, 512)], rs3, R_rs3, D)
                        with ExitStack() as es:
                            T.barrier()
                            ot = sb("ot", [128, 4, D], F32, es); R_ot = Res("ot"); es.callback(T.release, R_ot)
                            x1c = Slots(es, nc, "x1c", [128, 512], F32, 4)
                            fc = Slots(es, nc, "fc", [128, 512], F32, 4)
                            pto = PSlots(es, nc, "pto", [128, 4, 128], F32, 2)
                            for oc in range(KC):
                                x_t, rx = x1c.next()
                                T.dma("sp", x_t[:, :], x1T_scr[oc * 128:(oc + 1) * 128, ob:ob + 512], rx, reads=[R_x1T], writes=[rx])
                                f_t, rf = fc.next()
                                T.dma("sp", f_t[:, :], fT_scr[oc * 128:(oc + 1) * 128, :], rf, reads=[R_fT], writes=[rf])
                                T.op("dve", lambda e: e.scalar_tensor_tensor(out=f_t[:, :], in0=f_t[:, :], scalar=G2[:, oc:oc + 1], in1=rs3[:, :],
                                                                             op0=ALU.mult, op1=ALU.mult), [R_rs3, R_der], [rf])
                                T.op("dve", lambda e: e.tensor_tensor(out=f_t[:, :], in0=f_t[:, :], in1=x_t[:, :], op=ALU.add), [rx], [rf])
                                p_t, rp = pto.next()
                                T.group("pe", [(lambda e, tb=tb: e.transpose(out=p_t[:, tb, :], in_=f_t[:, tb * 128:(tb + 1) * 128], identity=ident[:, :]))
                                               for tb in range(4)], [rf, R_ident], [rp])
                                T.op("dve", lambda e: e.tensor_copy(out=ot[:, :, oc * 128:(oc + 1) * 128], in_=p_t[:, :, :]), [rp],
                                     [R_ot] if oc in (0, KC - 1) else [])
                            for tb in range(4):
                                row = 512 * r + tb * 128
                                T.dma("sp", out_d[row:row + 128, :], ot[:, tb, :], R_ot, reads=[R_ot], bulk=[R_out])
                T.final_wait("sp", R_out)
        return nc, T


def from_phase2(nc, T, L):
    pass


def _pack_vecs(inp, c_b, flag):
    def fm(v, nchunk):
        return np.ascontiguousarray(np.asarray(v, np.float32).reshape(nchunk, 128).T)
    cols = {
        "g_pre_mix": fm(inp["g_pre_mix"][0], 32), "g_post_mix": fm(inp["g_post_mix"][0], 32),
        "g_pre_ffn": fm(inp["g_pre_ffn"][0], 32), "g_post_ffn": fm(inp["g_post_ffn"][0], 32),
        "b_ada": fm(inp["b_ada"][0], 192),
        "conv_lru_w": np.concatenate([fm(inp["conv_lru_w"][0][j], 16) for j in range(4)], axis=1),
        "conv_lru_b": fm(inp["conv_lru_b"][0], 16), "b_rg_a": fm(inp["b_rg_a"][0], 16),
        "b_rg_x": fm(inp["b_rg_x"][0], 16), "lru_lambda": fm(inp["lru_lambda"][0], 16),
        "g_grp_lru": fm(inp["g_grp_lru"][0], 16), "g_grp_attn": fm(inp["g_grp_attn"][0], 16),
        "conv_ffn_w": np.concatenate([fm(inp["conv_ffn_w"][0][j], 172) for j in range(3)], axis=1),
        "conv_ffn_b": fm(inp["conv_ffn_b"][0], 172),
        "c": fm(c_b, 32), "flag": np.full((128, 1), flag, np.float32),
    }
    out = np.zeros((128, NVEC), np.float32)
    for n, (o, c) in VEC.items():
        assert cols[n].shape == (128, c), (n, cols[n].shape)
        out[:, o:o + c] = cols[n]
    return out


def make_in_maps(inp):
    x = np.asarray(inp["x"], np.float32)
    c = np.asarray(inp["c"], np.float32)
    ident = np.eye(128, dtype=np.float32)
    tri = np.where(np.arange(128)[None, :] <= np.arange(128)[:, None], 0.0, NEG).astype(np.float32)
    shared = {
        "ident": ident, "tri": tri,
        "w_ada": np.ascontiguousarray(np.asarray(inp["w_ada"], np.float32)[0]),
        "w_in": np.ascontiguousarray(np.asarray(inp["w_in"], np.float32)[0]),
        "w_rg_a": np.ascontiguousarray(np.asarray(inp["w_rg_a"], np.float32)[0]),
        "w_rg_x": np.ascontiguousarray(np.asarray(inp["w_rg_x"], np.float32)[0]),
        "w_out": np.ascontiguousarray(np.asarray(inp["w_out"], np.float32)[0]),
        "w_ffn_in": np.ascontiguousarray(np.asarray(inp["w_ffn_in"], np.float32)[0]),
        "w_ffn_out": np.ascontiguousarray(np.asarray(inp["w_ffn_out"], np.float32)[0]),
    }
    maps = []
    for core in range(8):
        b, h = core // 2, core % 2
        if h == 1:
            xwin = np.ascontiguousarray(x[b])
            kb = np.zeros((128, W), np.float32)
        else:
            xwin = np.concatenate([np.zeros((2048, D), np.float32), x[b, :2048]], axis=0)
            kb = np.zeros((128, W), np.float32)
            kb[:, :2048] = NEG
        m = dict(shared)
        m["xw"] = xwin
        m["kbias"] = kb
        m["vecs"] = _pack_vecs(inp, c[b], float(h))
        maps.append(m)
    return maps


def kernel(**inputs):
    nc, T = build()
    in_maps = make_in_maps(inputs)
    res = run_bass_kernel_spmd(nc, in_maps, core_ids=list(range(8)))
    out = np.zeros((4, 4096, D), np.float32)
    for core in range(8):
        b, h = core // 2, core % 2
        out[b, h * 2048:(h + 1) * 2048] = np.asarray(res.results[core]["out"], np.float32)
    return out
```

```python
import numpy as np
from contextlib import ExitStack
import concourse.bass as bass
import concourse.mybir as mybir
from concourse.bass_utils import run_bass_kernel_spmd

F32 = mybir.dt.float32
BF16 = mybir.dt.bfloat16
AF = mybir.ActivationFunctionType
ALU = mybir.AluOpType
AX = mybir.AxisListType

D = 4096
KC = 32
W = 4096
HALO0 = 1920
OWN0 = 2048
NOWN = W - HALO0
NQT = NOWN // 128
D_LRU = 2048
D_FF = 11008
NFC = D_FF // 128
D_IN = 9312
OFF_GATE, OFF_Q, OFF_K, OFF_V, OFF_QI, OFF_KI, OFF_WI = 2048, 4096, 6144, 6656, 7168, 9216, 9280
EPS = 1e-6
NEG = -1.0e30
TOPK = 256
NBIS = 16

VEC = {}
_off = 0
for _n, _c in [("g_pre_mix", 32), ("g_post_mix", 32), ("g_pre_ffn", 32), ("g_post_ffn", 32), ("b_ada", 192),
               ("conv_lru_w", 64), ("conv_lru_b", 16), ("b_rg_a", 16), ("b_rg_x", 16), ("lru_lambda", 16),
               ("g_grp_lru", 16), ("g_grp_attn", 16), ("conv_ffn_w", 516), ("conv_ffn_b", 172), ("c", 32),
               ("flag", 1)]:
    VEC[_n] = (_off, _c)
    _off += _c
NVEC = _off


class Res:
    __slots__ = ("name", "w", "r", "dsem", "dcount")

    def __init__(self, name):
        self.name = name
        self.w = {}
        self.r = {}
        self.dsem = None
        self.dcount = 0


class Trk:
    def __init__(self, nc):
        self.nc = nc
        self.eng = {"pe": nc.tensor, "act": nc.scalar, "dve": nc.vector, "pool": nc.gpsimd, "sp": nc.sync}
        self.sem = {}
        self.cnt = {}
        self.waited = {k: {} for k in self.eng}
        self.nsem = 0
        self.ninst = {k: 0 for k in self.eng}
        self.dslots = []
        self.dmax = {}
        self.free_d = []

    def release(self, res):
        if res.dsem is not None:
            self.free_d.append((res.dsem, res.dcount))
            res.dsem = None

    def barrier(self):
        deps = [(sem, self.cnt[e], e) for e, sem in self.sem.items()]
        deps += [(sem, cnt, "dma") for (sem, cnt) in self.dmax.values()]
        for e in self.eng:
            self._wait(e, [d for d in deps if d[2] != e or e != "pe"])

    def _newsem(self, tag):
        self.nsem += 1
        return self.nc.alloc_semaphore(f"{tag}{self.nsem}")

    def _wait(self, e, deps):
        best = {}
        for (sem, val, src) in deps:
            if src == "pe" and e == "pe":
                continue
            k = id(sem)
            if k not in best or best[k][1] < val:
                best[k] = (sem, val)
        wd = self.waited[e]
        for k, (sem, val) in best.items():
            if wd.get(k, 0) >= val:
                continue
            self.eng[e].wait_ge(sem, val)
            wd[k] = val

    @staticmethod
    def _deps(reads, writes, bulk=()):
        deps = []
        for r in reads:
            deps.extend(r.w.values())
        for w in writes:
            deps.extend(w.w.values())
            deps.extend(w.r.values())
        for b in bulk:
            deps.extend(b.r.values())
        return deps

    def _signal(self, e, inst, reads, writes):
        if e not in self.sem or self.cnt[e] >= 30000:
            self.sem[e] = self._newsem(e)
            self.cnt[e] = 0
        self.cnt[e] += 1
        inst.then_inc(self.sem[e], 1)
        ev = (self.sem[e], self.cnt[e], e)
        k = id(self.sem[e])
        for r in reads:
            r.r[k] = ev
        for w in writes:
            w.w = {k: ev}
            w.r = {}

    def op(self, e, fn, reads=(), writes=()):
        self._wait(e, self._deps(reads, writes))
        inst = fn(self.eng[e])
        self.ninst[e] += 1
        self._signal(e, inst, reads, writes)
        return inst

    def group(self, e, fns, reads=(), writes=()):
        self._wait(e, self._deps(reads, writes))
        inst = None
        for fn in fns:
            inst = fn(self.eng[e])
            self.ninst[e] += 1
        self._signal(e, inst, reads, writes)
        return inst

    def dma(self, q, out, in_, slot, reads=(), writes=(), bulk=()):
        self._wait(q, self._deps(reads, writes, bulk))
        if slot.dsem is None:
            if self.free_d:
                slot.dsem, slot.dcount = self.free_d.pop()
            else:
                slot.dsem = self._newsem("d")
        inst = self.eng[q].dma_start(out=out, in_=in_)
        self.ninst[q] += 1
        slot.dcount += 16
        inst.then_inc(slot.dsem, 16)
        self.dmax[id(slot.dsem)] = (slot.dsem, slot.dcount)
        ev = (slot.dsem, slot.dcount, "dma")
        k = id(slot.dsem)
        for r in reads:
            r.r[k] = ev
        for w in writes:
            w.w = {k: ev}
            w.r = {}
        for b in bulk:
            b.w[k] = ev
        return inst

    def final_wait(self, e, res):
        self._wait(e, self._deps([res], []))


_UID = [0]
_TRK = [None]


def _u(name):
    _UID[0] += 1
    return f"{name}_{_UID[0]}"


class Slots:
    def __init__(self, es, nc, name, shape, dtype, n):
        self.items = []
        for i in range(n):
            t = es.enter_context(nc.sbuf_tensor(_u(f"{name}{i}"), shape, dtype))
            rres = Res(f"{name}{i}")
            es.callback(_TRK[0].release, rres)
            self.items.append((t, rres))
        self.i = 0

    def next(self):
        it = self.items[self.i % len(self.items)]
        self.i += 1
        return it


class PSlots:
    def __init__(self, es, nc, name, shape, dtype, n):
        self.items = []
        for i in range(n):
            t = es.enter_context(nc.psum_tensor(_u(f"{name}{i}"), shape, dtype))
            self.items.append((t, Res(f"{name}{i}")))
        self.i = 0

    def next(self):
        it = self.items[self.i % len(self.items)]
        self.i += 1
        return it


def build(stop_after=99, dbg=False):
    nc = bass.Bass("TRN2", target_bir_lowering=False)
    T = Trk(nc)
    _TRK[0] = T

    def din(name, shape, dt=F32):
        return nc.dram_tensor(name, shape, dt, kind="ExternalInput").ap()

    def dscr(name, shape, dt=F32):
        kind = "ExternalOutput" if (dbg and name in dbg) else "Internal"
        return nc.dram_tensor(name, shape, dt, kind=kind).ap()

    xw = din("xw", [W, D])
    vecs_d = din("vecs", [128, NVEC])
    kbias_d = din("kbias", [128, W])
    ident_d = din("ident", [128, 128])
    tri_d = din("tri", [128, 128])
    w_ada = din("w_ada", [D, 6 * D])
    w_in = din("w_in", [D, D_IN])
    w_rg_a = din("w_rg_a", [16, 128, 128])
    w_rg_x = din("w_rg_x", [16, 128, 128])
    w_out = din("w_out", [D, D])
    w_f1 = din("w_ffn_in", [D, 2 * D_FF])
    w_f2 = din("w_ffn_out", [D_FF, D])
    out_d = nc.dram_tensor("out", [2048, D], F32, kind="ExternalOutput").ap()

    xl_scr = dscr("xl_scr", [D_LRU, W])
    gl_scr = dscr("gl_scr", [D_LRU, NOWN])
    q_scr = dscr("q_scr", [2048, NOWN], BF16)
    qi_scr = dscr("qi_scr", [2048, NOWN], BF16)
    kt_scr = dscr("kt_scr", [512, W], BF16)
    v_scr = dscr("v_scr", [W, 512], BF16)
    ki_scr = dscr("ki_scr", [128, W], BF16)
    xT_scr = dscr("xT_scr", [D, NOWN])
    mg_scr = dscr("mg_scr", [D, NOWN], BF16)
    x1T_scr = dscr("x1T_scr", [D, NOWN])
    fT_scr = dscr("fT_scr", [D, 512])
    R_xl, R_gl, R_q, R_qi, R_kt, R_v, R_ki, R_xT, R_mg, R_x1T, R_fT, R_out = [
        Res(n) for n in ("xl", "gl", "q", "qi", "kt", "v", "ki", "xT", "mg", "x1T", "fT", "out")]
    R_in = Res("inputs")

    es0 = ExitStack()
    with es0:
        sb = lambda name, shape, dt=F32, es=es0: es.enter_context(nc.sbuf_tensor(_u(name), shape, dt))
        vecs = sb("vecs_sb", [128, NVEC]); R_vecs = Res("vecs")
        ident = sb("ident_sb", [128, 128]); R_ident = Res("ident")
        identb = sb("identb", [128, 128], BF16); R_identb = Res("identb")
        onesb = sb("onesb", [128, 128], BF16); R_onesb = Res("onesb")
        one1 = sb("one1", [1, 1]); R_one1 = Res("one1")
        mod = sb("mod", [128, 192]); R_mod = Res("mod")
        A1 = sb("A1", [128, 32]); G1 = sb("G1", [128, 32]); A2 = sb("A2", [128, 32]); G2 = sb("G2", [128, 32])
        R_der = Res("derived")
        cneg = sb("cneg", [128, 16]); cneg2 = sb("cneg2", [128, 16])
        WI = sb("WI", [128, NQT, 32]); R_WI = Res("WI")

        def V(name, j=0, n=1):
            o, c = VEC[name]
            return vecs[:, o + j:o + j + n]

        T.dma("sp", vecs[:, :], vecs_d[:, :], R_vecs, reads=[R_in], writes=[R_vecs])
        T.dma("sp", ident[:, :], ident_d[:, :], R_ident, reads=[R_in], writes=[R_ident])
        T.op("dve", lambda e: e.tensor_copy(out=identb[:, :], in_=ident[:, :]), [R_ident], [R_identb])
        T.op("dve", lambda e: e.memset(onesb[:, :], 1.0), [], [R_onesb])
        T.op("dve", lambda e: e.memset(one1[:, :], 1.0), [], [R_one1])
        SHIFT1, SCALE1, GATE1, SHIFT2, SCALE2, GATE2 = [mod[:, 32 * i:32 * (i + 1)] for i in range(6)]

        with ExitStack() as es:
            silu = sb("silu", [128, 32], BF16, es); R_silu = Res("silu")
            wsl = Slots(es, nc, "wada", [128, KC, 512], BF16, 2)
            rowsb = Slots(es, nc, "rowsb", [1, 512], F32, 2)
            prow = PSlots(es, nc, "prow", [1, 512], F32, 2)
            pmod = es.enter_context(nc.psum_tensor(_u("pmod"), [128, 192], F32)); R_pmod = Res("pmod")
            T.op("act", lambda e: e.activation(out=silu[:, :], in_=V("c", 0, 32), func=AF.Silu), [R_vecs], [R_silu])
            wsrc = w_ada.rearrange("(kc p) c -> p kc c", p=128)
            for jc in range(48):
                wt, rw = wsl.next()
                T.dma("pool", wt[:, :, :], wsrc[:, :, jc * 512:(jc + 1) * 512], rw, reads=[R_in], writes=[rw])
                pr, rpr = prow.next()
                T.group("pe", [
                    (lambda e, kc=kc: e.matmul(pr[:, :], lhsT=silu[:, kc:kc + 1], rhs=wt[:, kc, :],
                                               start=(kc == 0), stop=(kc == KC - 1)))
                    for kc in range(KC)], [R_silu, rw], [rpr])
                rs, rrs = rowsb.next()
                T.op("act", lambda e: e.activation(out=rs[:, :], in_=pr[:, :], func=AF.Copy), [rpr], [rrs])
                T.group("pe", [
                    (lambda e, j=j: e.matmul(pmod[:, jc * 4 + j:jc * 4 + j + 1], lhsT=rs[0:1, j * 128:(j + 1) * 128],
                                             rhs=one1[0:1, 0:1], start=True, stop=True))
                    for j in range(4)], [rrs, R_one1], [R_pmod])
            T.op("dve", lambda e: e.tensor_tensor(out=mod[:, :], in0=pmod[:, :], in1=V("b_ada", 0, 192), op=ALU.add),
                 [R_pmod, R_vecs], [R_mod])
            tmp = sb("tmpder", [128, 32], F32, es); R_tmp = Res("tmpder")
            for (dst, sc, g) in ((A1, SCALE1, "g_pre_mix"), (A2, SCALE2, "g_pre_ffn")):
                T.op("dve", lambda e: e.tensor_scalar(out=tmp[:, :], in0=sc, scalar1=1.0, scalar2=None, op0=ALU.add),
                     [R_mod], [R_tmp])
                T.op("dve", lambda e: e.tensor_tensor(out=dst[:, :], in0=tmp[:, :], in1=V(g, 0, 32), op=ALU.mult),
                     [R_tmp, R_vecs, R_der], [R_der])
            for (dst, gt, g) in ((G1, GATE1, "g_post_mix"), (G2, GATE2, "g_post_ffn")):
                T.op("dve", lambda e: e.tensor_tensor(out=dst[:, :], in0=gt, in1=V(g, 0, 32), op=ALU.mult),
                     [R_mod, R_vecs, R_der], [R_der])
            t16 = sb("t16", [128, 16], F32, es); R_t16 = Res("t16")
            T.op("act", lambda e: e.activation(out=t16[:, :], in_=V("lru_lambda", 0, 16), func=AF.Exp, scale=-1.0),
                 [R_vecs], [R_t16])
            T.op("act", lambda e: e.activation(out=t16[:, :], in_=t16[:, :], func=AF.Ln, bias=1.0), [R_t16], [R_t16])
            T.op("dve", lambda e: e.tensor_scalar(out=cneg[:, :], in0=t16[:, :], scalar1=-8.0, scalar2=None,
                                                  op0=ALU.mult), [R_t16, R_der], [R_der])
            T.op("dve", lambda e: e.tensor_scalar(out=cneg2[:, :], in0=t16[:, :], scalar1=-16.0, scalar2=None,
                                                  op0=ALU.mult), [R_t16, R_der], [R_der])

        if stop_after >= 1:
            with ExitStack() as es:
                T.barrier()
                hT = sb("hT", [128, KC, 768], BF16, es)
                R_hT = [Res(f"hT{b}") for b in range(6)]
                wsl = Slots(es, nc, "win", [128, KC, 512], BF16, 2)
                xs = Slots(es, nc, "xs", [128, D], F32, 2)
                xn = Slots(es, nc, "xn", [128, D], BF16, 2)
                xTs = Slots(es, nc, "xTs", [128, KC, 128], F32, 1)
                st32 = Slots(es, nc, "st32", [128, 384], F32, 3)
                st16 = Slots(es, nc, "st16", [128, 384], BF16, 3)
                stv = Slots(es, nc, "stv", [128, 512], BF16, 2)
                ssb = Slots(es, nc, "ssb", [128, 2], F32, 2)
                ptr = PSlots(es, nc, "ptr", [128, 8, 128], BF16, 2)
                ptf = PSlots(es, nc, "ptf", [128, 4, 128], F32, 1)
                pmm = PSlots(es, nc, "pmm", [128, 512], F32, 4)
                wsrc = w_in.rearrange("(kc p) c -> p kc c", p=128)
                xT_dst = xT_scr.rearrange("(c p) t -> p c t", p=128)

                tiles = [(0, 768, False), (768, 768, False), (1536, 384, False),
                         (1920, 768, True), (2688, 768, True), (3456, 640, True)]
                for (t0, nt, own) in tiles:
                    nb = nt // 128
                    for b in range(nb):
                        tok = t0 + b * 128
                        x_t, rx = xs.next()
                        T.dma("sp", x_t[:, :], xw[tok:tok + 128, :], rx, reads=[R_in], writes=[rx])
                        xn_t, rxn = xn.next()
                        s_t, rs_ = ssb.next()
                        T.op("act", lambda e: e.activation(out=xn_t[:, :], in_=x_t[:, :], func=AF.Square,
                                                           accum_out=s_t[:, 0:1]), [rx], [rxn, rs_])
                        T.op("act", lambda e: e.activation(out=s_t[:, 1:2], in_=s_t[:, 0:1], func=AF.Ln,
                                                           scale=1.0 / D, bias=EPS), [rs_], [rs_])
                        T.op("act", lambda e: e.activation(out=s_t[:, 1:2], in_=s_t[:, 1:2], func=AF.Exp,
                                                           scale=-0.5), [rs_], [rs_])
                        T.op("dve", lambda e: e.tensor_scalar(out=xn_t[:, :], in0=x_t[:, :], scalar1=s_t[:, 1:2],
                                                              scalar2=None, op0=ALU.mult), [rx, rs_], [rxn])
                        for g8 in range(4):
                            p_t, rp = ptr.next()
                            T.group("pe", [
                                (lambda e, j=j: e.transpose(out=p_t[:, j, :],
                                                            in_=xn_t[:, (g8 * 8 + j) * 128:(g8 * 8 + j + 1) * 128],
                                                            identity=identb[:, :]))
                                for j in range(8)], [rxn, R_identb], [rp])
                            for j in range(8):
                                c = g8 * 8 + j
                                T.op("act", lambda e, c=c, j=j: e.activation(
                                    out=hT[:, c, b * 128:(b + 1) * 128], in_=p_t[:, j, :], func=AF.Identity,
                                    scale=A1[:, c:c + 1], bias=SHIFT1[:, c:c + 1]),
                                    [rp, R_der, R_mod], [R_hT[b]] if c in (0, 31) else [])
                        if own:
                            xt_t, rxt = xTs.next()
                            for g4 in range(8):
                                p_t, rp = ptf.next()
                                T.group("pe", [
                                    (lambda e, j=j: e.transpose(out=p_t[:, j, :],
                                                                in_=x_t[:, (g4 * 4 + j) * 128:(g4 * 4 + j + 1) * 128],
                                                                identity=ident[:, :]))
                                    for j in range(4)], [rx, R_ident], [rp])
                                T.op("dve", lambda e: e.tensor_copy(out=xt_t[:, g4 * 4:(g4 + 1) * 4, :],
                                                                    in_=p_t[:, :, :]), [rp], [rxt])
                            T.dma("sp", xT_dst[:, :, tok - HALO0:tok - HALO0 + 128], xt_t[:, :, :], rxt,
                                  reads=[rxt], bulk=[R_xT])
                    nsub = 2 if nt > 384 else 1
                    nsz = nt // nsub
                    subs = [(i * nsz, nsz) for i in range(nsub)]
                    R_hall = R_hT[:nb]
                    if own:
                        groups = [("xl", i) for i in range(4)] + [("gl", i) for i in range(4)] + \
                                 [("q", i) for i in range(4)] + [("k", 0), ("v", 0)] + \
                                 [("qi", i) for i in range(4)] + [("kw", 0)]
                    else:
                        groups = [("xl", i) for i in range(4)] + [("k", 0), ("v", 0), ("kw", 0)]
                    base = {"xl": 0, "gl": OFF_GATE, "q": OFF_Q, "k": OFF_K, "v": OFF_V, "qi": OFF_QI}
                    for (kind, gi) in groups:
                        wt, rw = wsl.next()
                        if kind == "kw":
                            T.dma("pool", wt[:, :, 0:64], wsrc[:, :, OFF_KI:OFF_KI + 64], rw, reads=[R_in], writes=[rw])
                            T.dma("pool", wt[:, :, 64:128], wsrc[:, :, OFF_KI:OFF_KI + 64], rw, reads=[R_in], bulk=[rw])
                            T.dma("pool", wt[:, :, 128:160], wsrc[:, :, OFF_WI:OFF_WI + 32], rw, reads=[R_in], bulk=[rw])
                        else:
                            c0 = base[kind] + gi * 512
                            T.dma("pool", wt[:, :, :], wsrc[:, :, c0:c0 + 512], rw, reads=[R_in], writes=[rw])
                        if kind == "v":
                            for b in range(nb):
                                p_t, rp = pmm.next()
                                T.group("pe", [
                                    (lambda e, kc=kc: e.matmul(p_t[:, :], lhsT=hT[:, kc, b * 128:(b + 1) * 128],
                                                               rhs=wt[:, kc, :], start=(kc == 0), stop=(kc == KC - 1)))
                                    for kc in range(KC)], [R_hT[b], rw], [rp])
                                s_t, rst = stv.next()
                                T.op("act", lambda e: e.activation(out=s_t[:, :], in_=p_t[:, :], func=AF.Copy), [rp], [rst])
                                T.dma("sp", v_scr[t0 + b * 128:t0 + (b + 1) * 128, :], s_t[:, :], rst, reads=[rst], bulk=[R_v])
                            continue
                        if kind == "kw":
                            for (s0, n) in subs:
                                p_t, rp = pmm.next()
                                T.group("pe", [
                                    (lambda e, kc=kc: e.matmul(p_t[:, 0:n], lhsT=wt[:, kc, 0:128], rhs=hT[:, kc, s0:s0 + n],
                                                               start=(kc == 0), stop=(kc == KC - 1)))
                                    for kc in range(KC)], R_hall + [rw], [rp])
                                s_t, rst = st16.next()
                                T.op("act", lambda e: e.activation(out=s_t[:, 0:n], in_=p_t[:, 0:n], func=AF.Copy), [rp], [rst])
                                T.dma("sp", ki_scr[:, t0 + s0:t0 + s0 + n], s_t[:, 0:n], rst, reads=[rst], bulk=[R_ki])
                            if own:
                                for b in range(nb):
                                    p_t, rp = pmm.next()
                                    T.group("pe", [
                                        (lambda e, kc=kc: e.matmul(p_t[:, 0:32], lhsT=hT[:, kc, b * 128:(b + 1) * 128],
                                                                   rhs=wt[:, kc, 128:160], start=(kc == 0), stop=(kc == KC - 1)))
                                        for kc in range(KC)], [R_hT[b], rw], [rp])
                                    qt = (t0 + b * 128 - HALO0) // 128
                                    T.op("act", lambda e: e.activation(out=WI[:, qt, :], in_=p_t[:, 0:32], func=AF.Copy),
                                         [rp], [R_WI])
                            continue
                        for j in range(4):
                            ch = gi * 4 + j
                            for (s0, n) in subs:
                                p_t, rp = pmm.next()
                                T.group("pe", [
                                    (lambda e, kc=kc: e.matmul(p_t[:, 0:n], lhsT=wt[:, kc, j * 128:(j + 1) * 128],
                                                               rhs=hT[:, kc, s0:s0 + n], start=(kc == 0), stop=(kc == KC - 1)))
                                    for kc in range(KC)], R_hall + [rw], [rp])
                                tw = t0 + s0
                                to = tw - HALO0
                                if kind == "xl":
                                    s_t, rst = st32.next()
                                    T.op("act", lambda e: e.activation(out=s_t[:, 0:n], in_=p_t[:, 0:n], func=AF.Copy), [rp], [rst])
                                    T.dma("sp", xl_scr[ch * 128:(ch + 1) * 128, tw:tw + n], s_t[:, 0:n], rst, reads=[rst], bulk=[R_xl])
                                elif kind == "gl":
                                    s_t, rst = st32.next()
                                    T.op("act", lambda e: e.activation(out=s_t[:, 0:n], in_=p_t[:, 0:n], func=AF.Gelu), [rp], [rst])
                                    T.dma("sp", gl_scr[ch * 128:(ch + 1) * 128, to:to + n], s_t[:, 0:n], rst, reads=[rst], bulk=[R_gl])
                                elif kind == "q":
                                    s_t, rst = st16.next()
                                    T.op("act", lambda e: e.activation(out=s_t[:, 0:n], in_=p_t[:, 0:n], func=AF.Copy,
                                                                       scale=float(128 ** -0.5)), [rp], [rst])
                                    T.dma("sp", q_scr[ch * 128:(ch + 1) * 128, to:to + n], s_t[:, 0:n], rst, reads=[rst], bulk=[R_q])
                                elif kind == "qi":
                                    s_t, rst = st16.next()
                                    T.op("act", lambda e: e.activation(out=s_t[:, 0:n], in_=p_t[:, 0:n], func=AF.Copy), [rp], [rst])
                                    T.dma("sp", qi_scr[ch * 128:(ch + 1) * 128, to:to + n], s_t[:, 0:n], rst, reads=[rst], bulk=[R_qi])
                                elif kind == "k":
                                    s_t, rst = st16.next()
                                    T.op("act", lambda e: e.activation(out=s_t[:, 0:n], in_=p_t[:, 0:n], func=AF.Copy), [rp], [rst])
                                    T.dma("sp", kt_scr[j * 128:(j + 1) * 128, tw:tw + n], s_t[:, 0:n], rst, reads=[rst], bulk=[R_kt])

        if stop_after >= 2:
            with ExitStack() as es:
                T.barrier()
                WA = sb("WA", [128, 16, 128], BF16, es); WX = sb("WX", [128, 16, 128], BF16, es)
                R_WA = Res("WA"); R_WX = Res("WX")
                T.dma("pool", WA[:, :, :], w_rg_a.rearrange("n i j -> i n j"), R_WA, reads=[R_in], writes=[R_WA])
                T.dma("pool", WX[:, :, :], w_rg_x.rearrange("n i j -> i n j"), R_WX, reads=[R_in], writes=[R_WX])
                xlb = sb("xlb", [128, 3 + W], F32, es); R_xlb = Res("xlb")
                carry = sb("carry", [128, 16], F32, es); R_carry = Res("carry")
                ssL = sb("ssL", [128, NOWN], F32, es); R_ssL = Res("ssL")
                YL = sb("YL", [128, 16, NOWN], BF16, es); R_YL = Res("YL")
                BL = 1024
                xc = Slots(es, nc, "xc", [128, BL], F32, 2)
                xcb = Slots(es, nc, "xcb", [128, BL], BF16, 2)
                rr = Slots(es, nc, "rr", [128, BL], F32, 2)
                ii = Slots(es, nc, "ii", [128, BL], F32, 2)
                aa = Slots(es, nc, "aa", [128, BL], F32, 2)
                s2 = Slots(es, nc, "s2", [128, BL], F32, 2)
                hb = Slots(es, nc, "hb", [128, BL], F32, 2)
                glt = Slots(es, nc, "glt", [128, BL], F32, 2)
                yy = Slots(es, nc, "yy", [128, BL], F32, 1)
                ysq = Slots(es, nc, "ysq", [128, BL], BF16, 1)
                pg = PSlots(es, nc, "pg", [128, 512], F32, 4)
                pss = PSlots(es, nc, "pss", [128, 512], F32, 2)
                T.op("dve", lambda e: e.memset(xlb[:, 0:3], 0.0), [], [R_xlb])
                T.op("dve", lambda e: e.memset(ssL[:, :], 0.0), [], [R_ssL])
                cw = lambda j, ch: V("conv_lru_w", j * 16 + ch)
                xlb2 = sb("xlb2", [128, 3 + W], F32, es); R_xlb2 = Res("xlb2")
                T.op("dve", lambda e: e.memset(xlb2[:, 0:3], 0.0), [], [R_xlb2])
                xlbs = [(xlb, R_xlb), (xlb2, R_xlb2)]
                for pr_ in range(8):
                    for k_ in range(2):
                        ch = 2 * pr_ + k_; xlb, R_xlb = xlbs[k_]
                        T.dma("sp", xlb[:, 3:3 + W], xl_scr[ch * 128:(ch + 1) * 128, :], R_xlb, reads=[R_xl], writes=[R_xlb])
                        T.op("dve", lambda e: e.tensor_scalar(out=xlb[:, OWN0:OWN0 + 3], in0=xlb[:, OWN0:OWN0 + 3],
                                                              scalar1=V("flag"), scalar2=None, op0=ALU.mult),
                             [R_vecs], [R_xlb])
                    for b in range(W // BL):
                        for k_ in range(2):
                            ch = 2 * pr_ + k_; xlb, R_xlb = xlbs[k_]
                            c0 = b * BL
                            xc_t, rxc = xc.next()
                            T.op("act", lambda e: e.activation(out=xc_t[:, :], in_=xlb[:, 3 + c0:3 + c0 + BL], func=AF.Identity,
                                                               scale=cw(3, ch), bias=V("conv_lru_b", ch)), [R_xlb, R_vecs], [rxc])
                            for j in (2, 1, 0):
                                T.op("dve", lambda e, j=j: e.scalar_tensor_tensor(
                                    out=xc_t[:, :], in0=xlb[:, j + c0:j + c0 + BL], scalar=cw(j, ch), in1=xc_t[:, :],
                                    op0=ALU.mult, op1=ALU.add), [R_xlb, R_vecs], [rxc])
                            xb_t, rxb = xcb.next()
                            T.op("act", lambda e: e.activation(out=xb_t[:, :], in_=xc_t[:, :], func=AF.Copy), [rxc], [rxb])
                            r_t, rr_ = rr.next(); i_t, ri_ = ii.next()
                            for sub in range(BL // 512):
                                sl = slice(sub * 512, (sub + 1) * 512)
                                pa, rpa = pg.next()
                                T.group("pe", [lambda e: e.matmul(pa[:, :], lhsT=WA[:, ch, :], rhs=xb_t[:, sl], start=True, stop=True)],
                                        [R_WA, rxb], [rpa])
                                T.op("act", lambda e: e.activation(out=r_t[:, sl], in_=pa[:, :], func=AF.Sigmoid,
                                                                   bias=V("b_rg_a", ch)), [rpa, R_vecs], [rr_])
                                px, rpx = pg.next()
                                T.group("pe", [lambda e: e.matmul(px[:, :], lhsT=WX[:, ch, :], rhs=xb_t[:, sl], start=True, stop=True)],
                                        [R_WX, rxb], [rpx])
                                T.op("act", lambda e: e.activation(out=i_t[:, sl], in_=px[:, :], func=AF.Sigmoid,
                                                                   bias=V("b_rg_x", ch)), [rpx, R_vecs], [ri_])
                            a_t, ra_ = aa.next(); s_t, rs_ = s2.next()
                            T.op("act", lambda e: e.activation(out=a_t[:, :], in_=r_t[:, :], func=AF.Exp, scale=cneg[:, ch:ch + 1]),
                                 [rr_, R_der], [ra_])
                            T.op("act", lambda e: e.activation(out=s_t[:, :], in_=r_t[:, :], func=AF.Exp, scale=cneg2[:, ch:ch + 1]),
                                 [rr_, R_der], [rs_])
                            T.op("act", lambda e: e.activation(out=s_t[:, :], in_=s_t[:, :], func=AF.Sqrt, scale=-1.0, bias=1.0),
                                 [rs_], [rs_])
                            T.op("dve", lambda e: e.tensor_tensor(out=i_t[:, :], in0=i_t[:, :], in1=xc_t[:, :], op=ALU.mult),
                                 [ri_, rxc], [ri_])
                            T.op("dve", lambda e: e.tensor_tensor(out=i_t[:, :], in0=i_t[:, :], in1=s_t[:, :], op=ALU.mult),
                                 [ri_, rs_], [ri_])
                            h_t, rh = hb.next()
                            init = 0.0 if b == 0 else carry[:, ch:ch + 1]
                            T.op("dve", lambda e: e.tensor_tensor_scan(out=h_t[:, :], data0=a_t[:, :], data1=i_t[:, :], initial=init,
                                                                       op0=ALU.mult, op1=ALU.add), [ra_, ri_, R_carry], [rh])
                            if c0 + BL == OWN0:
                                T.op("dve", lambda e: e.tensor_scalar(out=carry[:, ch:ch + 1], in0=h_t[:, BL - 1:BL], scalar1=V("flag"),
                                                                      scalar2=None, op0=ALU.mult), [rh, R_vecs], [R_carry])
                            else:
                                T.op("dve", lambda e: e.tensor_copy(out=carry[:, ch:ch + 1], in_=h_t[:, BL - 1:BL]), [rh], [R_carry])
                            lo = max(c0, HALO0)
                            if lo < c0 + BL:
                                n = c0 + BL - lo; off = lo - c0; orel = lo - HALO0
                                g_t, rg = glt.next()
                                T.dma("sp", g_t[:, 0:n], gl_scr[ch * 128:(ch + 1) * 128, orel:orel + n], rg, reads=[R_gl], writes=[rg])
                                y_t, ry = yy.next()
                                T.op("dve", lambda e: e.tensor_tensor(out=y_t[:, 0:n], in0=h_t[:, off:off + n], in1=g_t[:, 0:n], op=ALU.mult),
                                     [rh, rg], [ry])
                                T.op("act", lambda e: e.activation(out=YL[:, ch, orel:orel + n], in_=y_t[:, 0:n], func=AF.Copy), [ry], [R_YL])
                                q_t, rq = ysq.next()
                                T.op("act", lambda e: e.activation(out=q_t[:, 0:n], in_=y_t[:, 0:n], func=AF.Square), [ry], [rq])
                                for p0 in range(0, n, 512):
                                    pn = min(512, n - p0)
                                    ps_t, rps = pss.next()
                                    T.group("pe", [lambda e: e.matmul(ps_t[:, 0:pn], lhsT=onesb[:, :], rhs=q_t[:, p0:p0 + pn], start=True, stop=True)],
                                            [R_onesb, rq], [rps])
                                    T.op("dve", lambda e: e.tensor_tensor(out=ssL[:, orel + p0:orel + p0 + pn], in0=ps_t[:, 0:pn],
                                                                          in1=ssL[:, orel + p0:orel + p0 + pn], op=ALU.add), [rps], [R_ssL])
                T.op("act", lambda e: e.activation(out=ssL[:, :], in_=ssL[:, :], func=AF.Ln, scale=1.0 / D_LRU, bias=EPS), [R_ssL], [R_ssL])
                T.op("act", lambda e: e.activation(out=ssL[:, :], in_=ssL[:, :], func=AF.Exp, scale=-0.5), [R_ssL], [R_ssL])
                mgs = Slots(es, nc, "mgs", [128, NOWN], BF16, 2)
                for ch in range(16):
                    m_t, rm = mgs.next()
                    T.op("dve", lambda e: e.scalar_tensor_tensor(out=m_t[:, :], in0=YL[:, ch, :], scalar=V("g_grp_lru", ch), in1=ssL[:, :],
                                                                 op0=ALU.mult, op1=ALU.mult), [R_YL, R_ssL, R_vecs], [rm])
                    T.dma("sp", mg_scr[ch * 128:(ch + 1) * 128, :], m_t[:, :], rm, reads=[rm], bulk=[R_mg])

        if stop_after >= 3:
            with ExitStack() as es:
                T.barrier()
                KT = sb("KT", [128, 4, W], BF16, es); R_KT = Res("KT")
                Vv = sb("Vv", [128, 32, 512], BF16, es); R_Vv = Res("Vv")
                KI = sb("KI", [128, W], BF16, es); R_KI = Res("KI")
                kb_sb = sb("kb_sb", [128, W], BF16, es); R_kb = Res("kb")
                tri = sb("tri_sb", [128, 128], F32, es); R_tri = Res("tri")
                T.dma("sp", KI[:, :], ki_scr[:, :], R_KI, reads=[R_ki], writes=[R_KI])
                T.dma("pool", kb_sb[:, :], kbias_d[:, :], R_kb, reads=[R_in], writes=[R_kb])
                T.dma("sp", tri[:, :], tri_d[:, :], R_tri, reads=[R_in], writes=[R_tri])
                T.dma("sp", KT[:, :, :], kt_scr.rearrange("(g p) t -> p g t", p=128), R_KT, reads=[R_kt], writes=[R_KT])
                T.dma("sp", Vv[:, :, :], v_scr.rearrange("(b p) c -> p b c", p=128), R_Vv, reads=[R_v], writes=[R_Vv])
                for rr_ in (R_KT, R_Vv, R_KI, R_kb, R_tri):
                    es.callback(T.release, rr_)
                qiT = Slots(es, nc, "qiT", [128, 16, 128], BF16, 2)
                qT = Slots(es, nc, "qT", [128, 16, 128], BF16, 2)
                score = Slots(es, nc, "score", [128, W], F32, 2)
                Mt = Slots(es, nc, "Mt", [128, W], BF16, 2)
                MTs = Slots(es, nc, "MTs", [128, 32, 128], BF16, 2)
                rl = Slots(es, nc, "rl", [128, 512], BF16, 4)
                dgs = Slots(es, nc, "dgs", [128, 32, 128], BF16, 1)
                Pt = Slots(es, nc, "Pt", [128, 4, 128], BF16, 4)
                stt = Slots(es, nc, "stt", [128, 8], F32, 3)
                rinv = Slots(es, nc, "rinv", [128, 512], F32, 1)
                YAq = Slots(es, nc, "YAq", [128, 16, 128], F32, 1)
                ysqa = Slots(es, nc, "ysqa", [128, 512], BF16, 4)
                rab = Slots(es, nc, "rab", [128, 128], F32, 2)
                mst = Slots(es, nc, "mst", [128, 16, 128], BF16, 1)
                pd = PSlots(es, nc, "pd", [128, 512], F32, 2)
                pS = PSlots(es, nc, "pS", [128, 512], F32, 2)
                pO = PSlots(es, nc, "pO", [128, 512], F32, 1)
                pR = PSlots(es, nc, "pR", [128, 512], F32, 1)
                ptm = PSlots(es, nc, "ptm", [128, 8, 128], BF16, 1)
                psc = PSlots(es, nc, "psc", [128, 512], F32, 1)
                qi_src = qi_scr.rearrange("(c p) t -> p c t", p=128)
                q_src = q_scr.rearrange("(c p) t -> p c t", p=128)
                mg_dst = mg_scr.rearrange("(c p) t -> p c t", p=128)
                state = {}

                def stageA(j):
                    orel = j * 128
                    n = HALO0 + orel + 128
                    qi_t, rqi = qiT.next()
                    T.dma("sp", qi_t[:, :, :], qi_src[:, :, orel:orel + 128], rqi, reads=[R_qi], writes=[rqi])
                    sc_t, rsc = score.next()
                    state[("score", j)] = (sc_t, rsc)
                    items = [(s0, min(512, n - s0), hd) for s0 in range(0, n, 512) for hd in range(32)]
                    accst = {}
                    dg_t, rdg = dgs.next()
                    for hd_ in range(32):
                        T.op("dve", lambda e, hd_=hd_: e.tensor_scalar(out=dg_t[:, hd_, :], in0=identb[:, :], scalar1=WI[:, j, hd_:hd_ + 1],
                                                                        scalar2=None, op0=ALU.mult),
                             [R_identb, R_WI], [rdg] if hd_ in (0, 31) else [])
                    NI = len(items)
                    pts = {}
                    rts = {}

                    def mm(t):
                        s0, ns, hd = items[t]
                        c, hh = hd // 2, hd % 2
                        p_t, rp = pd.next()
                        pts[t] = (p_t, rp)
                        T.group("pe", [lambda e: e.matmul(p_t[:, 0:ns], lhsT=qi_t[hh * 64:(hh + 1) * 64, c, :],
                                                          rhs=KI[hh * 64:(hh + 1) * 64, s0:s0 + ns], start=True, stop=True)],
                                [rqi, R_KI], [rp])

                    def relu(t):
                        s0, ns, hd = items[t]
                        p_t, rp = pts.pop(t)
                        r_t, rr_ = rl.next()
                        rts[t] = (r_t, rr_)
                        T.op("act", lambda e: e.activation(out=r_t[:, 0:ns], in_=p_t[:, 0:ns], func=AF.Relu), [rp], [rr_])

                    def acc(t):
                        s0, ns, hd = items[t]
                        r_t, rr_ = rts.pop(t)
                        if hd == 0:
                            accst["psc"] = psc.next()
                        ps_t, rps = accst["psc"]
                        T.group("pe", [lambda e: e.matmul(ps_t[:, 0:ns], lhsT=dg_t[:, hd, :], rhs=r_t[:, 0:ns], start=(hd == 0), stop=(hd == 31))],
                                [rdg, rr_], [rps])
                        if hd == 31:
                            T.op("dve", lambda e: e.tensor_copy(out=sc_t[:, s0:s0 + ns], in_=ps_t[:, 0:ns]), [rps], [rsc])

                    mm(0)
                    for t in range(NI + 1):
                        if t + 1 < NI:
                            mm(t + 1)
                        if t < NI:
                            relu(t)
                        if t >= 2:
                            acc(t - 2)
                        yield
                    acc(NI - 1)

                def stageB(j):
                    orel = j * 128
                    n = HALO0 + orel + 128
                    nkb = n // 128
                    sc_t, rsc = state.pop(("score", j))
                    st_t, rst = stt.next()
                    LO, HI, MID, CNT, PW, RNG = [st_t[:, k:k + 1] for k in range(6)]
                    T.op("dve", lambda e: e.tensor_reduce(out=LO, in_=sc_t[:, 0:n], axis=AX.X, op=ALU.min), [rsc], [rst])
                    T.op("dve", lambda e: e.tensor_reduce(out=HI, in_=sc_t[:, 0:n], axis=AX.X, op=ALU.max), [rsc], [rst])
                    yield
                    T.op("dve", lambda e: e.tensor_tensor(out=RNG, in0=HI, in1=LO, op=ALU.subtract), [], [rst])
                    T.op("dve", lambda e: e.tensor_tensor(out=sc_t[:, 0:n], in0=sc_t[:, 0:n], in1=kb_sb[:, 0:n], op=ALU.add), [R_kb], [rsc])
                    T.op("dve", lambda e: e.tensor_tensor(out=sc_t[:, n - 128:n], in0=sc_t[:, n - 128:n], in1=tri[:, :], op=ALU.add),
                         [R_tri], [rsc])
                    yield
                    m_t, rm = Mt.next()
                    for it in range(NBIS):
                        wk = float(2.0 ** -(it + 1))
                        T.op("dve", lambda e: e.tensor_scalar(out=MID, in0=RNG, scalar1=wk, scalar2=LO, op0=ALU.mult, op1=ALU.add), [], [rst])
                        T.op("dve", lambda e: e.tensor_scalar(out=m_t[:, 0:n], in0=sc_t[:, 0:n], scalar1=MID, scalar2=0.0,
                                                              op0=ALU.is_ge, op1=ALU.add, accum_out=CNT), [rsc], [rm, rst])
                        T.op("dve", lambda e: e.tensor_scalar(out=PW, in0=CNT, scalar1=float(TOPK), scalar2=wk, op0=ALU.is_ge, op1=ALU.mult),
                             [], [rst])
                        T.op("dve", lambda e: e.scalar_tensor_tensor(out=LO, in0=PW, scalar=RNG, in1=LO, op0=ALU.mult, op1=ALU.add), [], [rst])
                        yield
                    T.op("dve", lambda e: e.tensor_scalar(out=LO, in0=LO, scalar1=-1.0e29, scalar2=None, op0=ALU.max), [], [rst])
                    T.op("dve", lambda e: e.tensor_scalar(out=m_t[:, 0:n], in0=sc_t[:, 0:n], scalar1=LO, scalar2=None, op0=ALU.is_ge),
                         [rsc, rst], [rm])
                    yield
                    mt_t, rmt = MTs.next()
                    state[("mt", j)] = (mt_t, rmt)
                    for k0 in range(0, nkb, 8):
                        m8 = min(8, nkb - k0)
                        p_t, rp = ptm.next()
                        T.group("pe", [(lambda e, i=i: e.transpose(out=p_t[:, i, :], in_=m_t[:, (k0 + i) * 128:(k0 + i + 1) * 128],
                                                                   identity=identb[:, :])) for i in range(m8)], [rm, R_identb], [rp])
                        T.op("act", lambda e: e.activation(out=mt_t[:, k0:k0 + m8, :], in_=p_t[:, 0:m8, :], func=AF.Copy), [rp], [rmt])
                        yield

                def stageC(j):
                    orel = j * 128
                    n = HALO0 + orel + 128
                    nkb = n // 128
                    mt_t, rmt = state.pop(("mt", j))
                    q_t, rq = qT.next()
                    T.dma("sp", q_t[:, :, :], q_src[:, :, orel:orel + 128], rq, reads=[R_q], writes=[rq])
                    ya_t, rya = YAq.next()
                    yqs = []
                    steps = [(g, kb) for g in range(4) for kb in range(nkb)]
                    NS = len(steps)
                    sts = {}
                    pps = {}
                    accs = {}

                    def smm(t):
                        g, kb = steps[t]
                        s_t, rs_ = pS.next()
                        sts[t] = (s_t, rs_)
                        T.group("pe", [lambda e: e.matmul(s_t[:, :], lhsT=KT[:, g, kb * 128:(kb + 1) * 128],
                                                          rhs=q_t[:, 4 * g:4 * g + 4, :], start=True, stop=True)], [R_KT, rq], [rs_])

                    def expmask(t):
                        g, kb = steps[t]
                        s_t, rs_ = sts.pop(t)
                        p_t, rp = Pt.next()
                        pps[t] = (p_t, rp)
                        T.op("act", lambda e: e.activation(out=p_t[:, :, :], in_=s_t[:, :], func=AF.Exp), [rs_], [rp])
                        T.op("pool", lambda e: e.tensor_tensor(out=p_t[:, :, :], in0=p_t[:, :, :],
                                                               in1=mt_t[:, kb:kb + 1, :].broadcast_to([128, 4, 128]), op=ALU.mult), [rmt], [rp])

                    def pv(t):
                        g, kb = steps[t]
                        p_t, rp = pps.pop(t)
                        if kb == 0:
                            accs[g] = (pO.next(), pR.next())
                        (o_t, ro), (rr_t, rrr) = accs[g]
                        T.group("pe", [
                            lambda e: e.matmul(o_t[:, :], lhsT=Vv[:, kb, g * 128:(g + 1) * 128], rhs=p_t[:, :, :], start=(kb == 0), stop=(kb == nkb - 1)),
                            lambda e: e.matmul(rr_t[:, :], lhsT=onesb[:, :], rhs=p_t[:, :, :], start=(kb == 0), stop=(kb == nkb - 1))],
                            [R_Vv, R_onesb, rp], [ro, rrr])
                        if kb == nkb - 1:
                            ri_t, rri = rinv.next()
                            T.op("act", lambda e: e.activation(out=ri_t[:, :], in_=rr_t[:, :], func=AF.Ln, bias=1.0e-30), [rrr], [rri])
                            T.op("act", lambda e: e.activation(out=ri_t[:, :], in_=ri_t[:, :], func=AF.Exp, scale=-1.0), [], [rri])
                            T.op("dve", lambda e: e.tensor_tensor(out=ya_t[:, 4 * g:4 * g + 4, :], in0=o_t[:, :], in1=ri_t[:, :], op=ALU.mult),
                                 [ro, rri], [rya])
                            yq_t, ryq = ysqa.next()
                            T.op("act", lambda e: e.activation(out=yq_t[:, :], in_=ya_t[:, 4 * g:4 * g + 4, :], func=AF.Square), [rya], [ryq])
                            yqs.append((yq_t, ryq))

                    smm(0)
                    for t in range(NS + 1):
                        if t + 1 < NS:
                            smm(t + 1)
                        if t < NS:
                            expmask(t)
                        if t >= 2:
                            pv(t - 2)
                        yield
                    pv(NS - 1)
                    pss_full, rpss = pR.next()
                    pss_t = pss_full[:, 0:128]
                    T.group("pe", [(lambda e, gi=gi, h4=h4: e.matmul(pss_t, lhsT=onesb[:, :], rhs=yqs[gi][0][:, h4 * 128:(h4 + 1) * 128],
                                                                     start=(gi == 0 and h4 == 0), stop=(gi == 3 and h4 == 3)))
                                   for gi in range(4) for h4 in range(4)], [R_onesb] + [y[1] for y in yqs], [rpss])
                    ra_t, rra = rab.next()
                    T.op("act", lambda e: e.activation(out=ra_t[:, :], in_=pss_t, func=AF.Ln, scale=1.0 / 2048, bias=EPS), [rpss], [rra])
                    T.op("act", lambda e: e.activation(out=ra_t[:, :], in_=ra_t[:, :], func=AF.Exp, scale=-0.5), [], [rra])
                    ms_t, rms = mst.next()
                    for hd in range(16):
                        T.op("dve", lambda e: e.scalar_tensor_tensor(out=ms_t[:, hd, :], in0=ya_t[:, hd, :], scalar=V("g_grp_attn", hd),
                                                                     in1=ra_t[:, :], op0=ALU.mult, op1=ALU.mult),
                             [rya, rra, R_vecs], [rms] if hd in (0, 15) else [])
                    T.dma("sp", mg_dst[:, 16:32, orel:orel + 128], ms_t[:, :, :], rms, reads=[rms], bulk=[R_mg])
                    yield

                def nyields(kind, j):
                    n = HALO0 + j * 128 + 128
                    if kind == "A":
                        return 32 * ((n + 511) // 512)
                    if kind == "B":
                        return NBIS + 3 + (n // 128 + 7) // 8
                    return 4 * (n // 128) + 5

                for step in range(NQT + 2):
                    gens = []
                    for kind, fn, jj in (("A", stageA, step), ("B", stageB, step - 1), ("C", stageC, step - 2)):
                        if 0 <= jj < NQT:
                            gens.append([fn(jj), 0, nyields(kind, jj)])
                    while gens:
                        gens.sort(key=lambda g: g[1] / g[2])
                        g = gens[0]
                        try:
                            next(g[0])
                            g[1] += 1
                        except StopIteration:
                            gens.remove(g)

        if stop_after >= 4:
            with ExitStack() as es4:
                T.barrier()
                fcar = sb("fcar", [128, 2 * NFC, 2], F32, es4); R_fcar = Res("fcar")
                T.op("dve", lambda e: e.memset(fcar[:, :, :], 0.0), [], [R_fcar])
                mg_src = mg_scr.rearrange("(c p) t -> p c t", p=128)
                wo_src = w_out.rearrange("(kc p) c -> p kc c", p=128)
                w1_src = w_f1.rearrange("(kc p) c -> p kc c", p=128)
                w2_src = w_f2.rearrange("(c p) o -> p c o", p=128)
                fw = lambda j, cg: V("conv_ffn_w", j * 172 + cg)

                def rstd_from(ps_list, subs, dst, rdst, nfeat):
                    for (p_t, rp), (l0, n) in zip(ps_list, subs):
                        T.op("act", lambda e: e.activation(out=dst[:, l0:l0 + n], in_=p_t[:, 0:n], func=AF.Ln, scale=1.0 / nfeat, bias=EPS),
                             [rp], [rdst])
                    T.op("act", lambda e: e.activation(out=dst[:, :], in_=dst[:, :], func=AF.Exp, scale=-0.5), [], [rdst])

                for r in range(4):
                    if r == 0:
                        o_start, nr, subs = 126, 514, [(0, 2), (2, 512)]
                    else:
                        o_start, nr, subs = 128 + 512 * r, 512, [(0, 512)]
                    ob = 128 + 512 * r
                    with ExitStack() as esr:
                        T.barrier()
                        h2T = sb("h2T", [128, KC, 514], BF16, esr); R_h2T = Res("h2T")
                        rs3 = sb("rs3", [128, 512], F32, esr); R_rs3 = Res("rs3")
                        with ExitStack() as es:
                            mgT = sb("mgT", [128, KC, 514], BF16, es); R_mgT = Res("mgT"); es.callback(T.release, R_mgT)
                            mixT = sb("mixT", [128, KC, 514], F32, es); R_mix = [Res(f"mix{c}") for c in range(KC)]
                            rs1 = sb("rs1", [128, 514], F32, es); R_rs1 = Res("rs1")
                            S_x1 = Res("x1store"); es.callback(T.release, S_x1)
                            rs2 = sb("rs2", [128, 514], F32, es); R_rs2 = Res("rs2")
                            wo = Slots(es, nc, "wo", [128, KC, 256], BF16, 2)
                            sq = Slots(es, nc, "sq", [128, 514], BF16, 3)
                            xTc = Slots(es, nc, "xTc", [128, 514], F32, 4)
                            tmpf = Slots(es, nc, "tmpf", [128, 514], F32, 3)
                            pm = PSlots(es, nc, "pm", [128, 512], F32, 3)
                            pq1 = [PSlots(es, nc, f"pq1{i}", [128, 512], F32, 1).next() for i in range(len(subs))]
                            pq2 = [PSlots(es, nc, f"pq2{i}", [128, 512], F32, 1).next() for i in range(len(subs))]
                            T.dma("sp", mgT[:, :, 0:nr], mg_src[:, :, o_start:o_start + nr], R_mgT, reads=[R_mg], writes=[R_mgT])
                            for og in range(16):
                                wt, rw = wo.next()
                                T.dma("pool", wt[:, :, :], wo_src[:, :, og * 256:(og + 1) * 256], rw, reads=[R_in], writes=[rw])
                                for jj in range(2):
                                    oc = og * 2 + jj
                                    for si, (l0, n) in enumerate(subs):
                                        p_t, rp = pm.next()
                                        T.group("pe", [(lambda e, kc=kc: e.matmul(p_t[:, 0:n], lhsT=wt[:, kc, jj * 128:(jj + 1) * 128],
                                                                                  rhs=mgT[:, kc, l0:l0 + n], start=(kc == 0), stop=(kc == KC - 1)))
                                                       for kc in range(KC)], [rw, R_mgT], [rp])
                                        T.op("act", lambda e: e.activation(out=mixT[:, oc, l0:l0 + n], in_=p_t[:, 0:n], func=AF.Copy),
                                             [rp], [R_mix[oc]])
                                        q_t, rq_ = sq.next()
                                        T.op("act", lambda e: e.activation(out=q_t[:, 0:n], in_=p_t[:, 0:n], func=AF.Square), [rp], [rq_])
                                        T.group("pe", [lambda e: e.matmul(pq1[si][0][:, 0:n], lhsT=onesb[:, :], rhs=q_t[:, 0:n],
                                                                          start=(oc == 0), stop=(oc == KC - 1))], [R_onesb, rq_], [pq1[si][1]])
                            rstd_from(pq1, subs, rs1, R_rs1, D)
                            xq = []

                            def ldx(oc):
                                x_t, rx = xTc.next()
                                T.dma("sp", x_t[:, 0:nr], xT_scr[oc * 128:(oc + 1) * 128, o_start:o_start + nr], rx, reads=[R_xT], writes=[rx])
                                xq.append((x_t, rx))
                            for oc in range(3):
                                ldx(oc)
                            for oc in range(KC):
                                if oc + 3 < KC:
                                    ldx(oc + 3)
                                x_t, rx = xq.pop(0)
                                T.op("dve", lambda e: e.scalar_tensor_tensor(out=mixT[:, oc, 0:nr], in0=mixT[:, oc, 0:nr], scalar=G1[:, oc:oc + 1],
                                                                             in1=rs1[:, 0:nr], op0=ALU.mult, op1=ALU.mult),
                                     [R_rs1, R_der], [R_mix[oc]])
                                T.op("dve", lambda e: e.tensor_tensor(out=mixT[:, oc, 0:nr], in0=mixT[:, oc, 0:nr], in1=x_t[:, 0:nr], op=ALU.add),
                                     [rx], [R_mix[oc]])
                                T.dma("sp", x1T_scr[oc * 128:(oc + 1) * 128, o_start:o_start + nr], mixT[:, oc, 0:nr], S_x1,
                                      reads=[R_mix[oc]], bulk=[R_x1T])
                                for si, (l0, n) in enumerate(subs):
                                    q_t, rq_ = sq.next()
                                    T.op("act", lambda e: e.activation(out=q_t[:, 0:n], in_=mixT[:, oc, l0:l0 + n], func=AF.Square),
                                         [R_mix[oc]], [rq_])
                                    T.group("pe", [lambda e: e.matmul(pq2[si][0][:, 0:n], lhsT=onesb[:, :], rhs=q_t[:, 0:n],
                                                                      start=(oc == 0), stop=(oc == KC - 1))], [R_onesb, rq_], [pq2[si][1]])
                            rstd_from(pq2, subs, rs2, R_rs2, D)
                            for oc in range(KC):
                                t_t, rt = tmpf.next()
                                T.op("dve", lambda e: e.scalar_tensor_tensor(out=t_t[:, 0:nr], in0=mixT[:, oc, 0:nr], scalar=A2[:, oc:oc + 1],
                                                                             in1=rs2[:, 0:nr], op0=ALU.mult, op1=ALU.mult),
                                     [R_mix[oc], R_rs2, R_der], [rt])
                                T.op("act", lambda e: e.activation(out=h2T[:, oc, 0:nr], in_=t_t[:, 0:nr], func=AF.Identity,
                                                                   bias=SHIFT2[:, oc:oc + 1]), [rt, R_mod], [R_h2T] if oc in (0, KC - 1) else [])
                            if r == 0:
                                T.op("dve", lambda e: e.tensor_scalar(out=h2T[:, :, 0:2], in0=h2T[:, :, 0:2], scalar1=V("flag"), scalar2=None,
                                                                      op0=ALU.mult), [R_vecs], [R_h2T])
                        with ExitStack() as esf:
                            T.barrier()
                            actT = sb("actT", [128, NFC, 512], BF16, esf); R_act = Res("actT")
                            hoff = nr - 512
                            with ExitStack() as es:
                                w1 = Slots(es, nc, "w1", [128, KC, 512], BF16, 2)
                                yc = Slots(es, nc, "yc", [128, 514], F32, 2)
                                uu = Slots(es, nc, "uu", [128, 512], F32, 4)
                                py = PSlots(es, nc, "py", [128, 512], F32, 4)
                                for gi in range(NFC // 2):
                                    c0 = gi * 2
                                    wt, rw = w1.next()
                                    T.dma("pool", wt[:, :, 0:256], w1_src[:, :, c0 * 128:c0 * 128 + 256], rw, reads=[R_in], writes=[rw])
                                    T.dma("pool", wt[:, :, 256:512], w1_src[:, :, D_FF + c0 * 128:D_FF + c0 * 128 + 256], rw, reads=[R_in], bulk=[rw])
                                    for pr in range(2):
                                        us = []
                                        for i in (pr, pr + 2):
                                            cg = c0 + i if i < 2 else NFC + c0 + i - 2
                                            u_t, ru = uu.next()
                                            for (l0, n) in subs:
                                                p_t, rp = py.next()
                                                T.group("pe", [(lambda e, kc=kc: e.matmul(p_t[:, 0:n], lhsT=wt[:, kc, i * 128:(i + 1) * 128],
                                                                                          rhs=h2T[:, kc, l0:l0 + n], start=(kc == 0), stop=(kc == KC - 1)))
                                                               for kc in range(KC)], [rw, R_h2T], [rp])
                                                if n == 2:
                                                    T.op("dve", lambda e: e.tensor_copy(out=fcar[:, cg, :], in_=p_t[:, 0:2]), [rp], [R_fcar])
                                                    continue
                                                y_t, ry = yc.next()
                                                T.op("dve", lambda e: e.tensor_copy(out=y_t[:, 0:2], in_=fcar[:, cg, :]), [R_fcar], [ry])
                                                T.op("act", lambda e: e.activation(out=y_t[:, 2:514], in_=p_t[:, 0:512], func=AF.Copy), [rp], [ry])
                                                T.op("act", lambda e: e.activation(out=u_t[:, :], in_=p_t[:, 0:512], func=AF.Identity,
                                                                                   scale=fw(2, cg), bias=V("conv_ffn_b", cg)), [rp, R_vecs], [ru])
                                                T.op("dve", lambda e: e.scalar_tensor_tensor(out=u_t[:, :], in0=y_t[:, 1:513], scalar=fw(1, cg), in1=u_t[:, :],
                                                                                             op0=ALU.mult, op1=ALU.add), [ry, R_vecs], [ru])
                                                T.op("dve", lambda e: e.scalar_tensor_tensor(out=u_t[:, :], in0=y_t[:, 0:512], scalar=fw(0, cg), in1=u_t[:, :],
                                                                                             op0=ALU.mult, op1=ALU.add), [ry, R_vecs], [ru])
                                                T.op("dve", lambda e: e.tensor_copy(out=fcar[:, cg, :], in_=y_t[:, 512:514]), [ry], [R_fcar])
                                            us.append((u_t, ru))
                                        (ug, rug), (uu_, ruu) = us
                                        T.op("act", lambda e: e.activation(out=ug[:, :], in_=ug[:, :], func=AF.Gelu), [], [rug])
                                        T.op("dve", lambda e: e.tensor_tensor(out=actT[:, c0 + pr, :], in0=ug[:, :], in1=uu_[:, :], op=ALU.mult),
                                             [rug, ruu], [R_act])
                            with ExitStack() as es:
                                T.barrier()
                                w2 = Slots(es, nc, "w2", [128, 22, 512], BF16, 3)
                                fst = Slots(es, nc, "fst", [128, 512], F32, 3)
                                sqf = Slots(es, nc, "sqf", [128, 512], BF16, 2)
                                pf = [PSlots(es, nc, f"pf{j}", [128, 512], F32, 1).next() for j in range(4)]
                                pq3 = PSlots(es, nc, "pq3", [128, 512], F32, 1).next()
                                blocks = [(0, 22), (22, 22), (44, 22), (66, 20)]
                                for og in range(8):
                                    for (cb0, nb) in blocks:
                                        wt, rw = w2.next()
                                        T.dma("pool", wt[:, 0:nb, :], w2_src[:, cb0:cb0 + nb, og * 512:(og + 1) * 512], rw, reads=[R_in], writes=[rw])
                                        T.group("pe", [(lambda e, cl=cl, j=j: e.matmul(pf[j][0][:, :], lhsT=wt[:, cl, j * 128:(j + 1) * 128],
                                                                                       rhs=actT[:, cb0 + cl, :], start=(cb0 + cl == 0),
                                                                                       stop=(cb0 + cl == NFC - 1)))
                                                       for cl in range(nb) for j in range(4)], [rw, R_act], [pf[j][1] for j in range(4)])
                                    for j in range(4):
                                        oc = og * 4 + j
                                        f_t, rf = fst.next()
                                        T.op("act", lambda e: e.activation(out=f_t[:, :], in_=pf[j][0][:, :], func=AF.Copy), [pf[j][1]], [rf])
                                        T.dma("sp", fT_scr[oc * 128:(oc + 1) * 128, :], f_t[:, :], rf, reads=[rf], bulk=[R_fT])
                                        q_t, rq_ = sqf.next()
                                        T.op("act", lambda e: e.activation(out=q_t[:, :], in_=pf[j][0][:, :], func=AF.Square), [pf[j][1]], [rq_])
                                        T.group("pe", [lambda e: e.matmul(pq3[0][:, :], lhsT=onesb[:, :], rhs=q_t[:, :], start=(oc == 0), stop=(oc == KC - 1))],
                                                [R_onesb, rq_], [pq3[1]])
                                rstd_from([pq3], [(0, 512)], rs3, R_rs3, D)
                        with ExitStack() as es:
                            T.barrier()
                            ot = sb("ot", [128, 4, D], F32, es); R_ot = Res("ot"); es.callback(T.release, R_ot)
                            x1c = Slots(es, nc, "x1c", [128, 512], F32, 4)
                            fc = Slots(es, nc, "fc", [128, 512], F32, 4)
                            pto = PSlots(es, nc, "pto", [128, 4, 128], F32, 2)
                            for oc in range(KC):
                                x_t, rx = x1c.next()
                                T.dma("sp", x_t[:, :], x1T_scr[oc * 128:(oc + 1) * 128, ob:ob + 512], rx, reads=[R_x1T], writes=[rx])
                                f_t, rf = fc.next()
                                T.dma("sp", f_t[:, :], fT_scr[oc * 128:(oc + 1) * 128, :], rf, reads=[R_fT], writes=[rf])
                                T.op("dve", lambda e: e.scalar_tensor_tensor(out=f_t[:, :], in0=f_t[:, :], scalar=G2[:, oc:oc + 1], in1=rs3[:, :],
                                                                             op0=ALU.mult, op1=ALU.mult), [R_rs3, R_der], [rf])
                                T.op("dve", lambda e: e.tensor_tensor(out=f_t[:, :], in0=f_t[:, :], in1=x_t[:, :], op=ALU.add), [rx], [rf])
                                p_t, rp = pto.next()
                                T.group("pe", [(lambda e, tb=tb: e.transpose(out=p_t[:, tb, :], in_=f_t[:, tb * 128:(tb + 1) * 128], identity=ident[:, :]))
                                               for tb in range(4)], [rf, R_ident], [rp])
                                T.op("dve", lambda e: e.tensor_copy(out=ot[:, :, oc * 128:(oc + 1) * 128], in_=p_t[:, :, :]), [rp],
                                     [R_ot] if oc in (0, KC - 1) else [])
                            for tb in range(4):
                                row = 512 * r + tb * 128
                                T.dma("sp", out_d[row:row + 128, :], ot[:, tb, :], R_ot, reads=[R_ot], bulk=[R_out])
                T.final_wait("sp", R_out)
        return nc, T


def from_phase2(nc, T, L):
    pass


def _pack_vecs(inp, c_b, flag):
    def fm(v, nchunk):
        return np.ascontiguousarray(np.asarray(v, np.float32).reshape(nchunk, 128).T)
    cols = {
        "g_pre_mix": fm(inp["g_pre_mix"][0], 32), "g_post_mix": fm(inp["g_post_mix"][0], 32),
        "g_pre_ffn": fm(inp["g_pre_ffn"][0], 32), "g_post_ffn": fm(inp["g_post_ffn"][0], 32),
        "b_ada": fm(inp["b_ada"][0], 192),
        "conv_lru_w": np.concatenate([fm(inp["conv_lru_w"][0][j], 16) for j in range(4)], axis=1),
        "conv_lru_b": fm(inp["conv_lru_b"][0], 16), "b_rg_a": fm(inp["b_rg_a"][0], 16),
        "b_rg_x": fm(inp["b_rg_x"][0], 16), "lru_lambda": fm(inp["lru_lambda"][0], 16),
        "g_grp_lru": fm(inp["g_grp_lru"][0], 16), "g_grp_attn": fm(inp["g_grp_attn"][0], 16),
        "conv_ffn_w": np.concatenate([fm(inp["conv_ffn_w"][0][j], 172) for j in range(3)], axis=1),
        "conv_ffn_b": fm(inp["conv_ffn_b"][0], 172),
        "c": fm(c_b, 32), "flag": np.full((128, 1), flag, np.float32),
    }
    out = np.zeros((128, NVEC), np.float32)
    for n, (o, c) in VEC.items():
        assert cols[n].shape == (128, c), (n, cols[n].shape)
        out[:, o:o + c] = cols[n]
    return out


def make_in_maps(inp):
    x = np.asarray(inp["x"], np.float32)
    c = np.asarray(inp["c"], np.float32)
    ident = np.eye(128, dtype=np.float32)
    tri = np.where(np.arange(128)[None, :] <= np.arange(128)[:, None], 0.0, NEG).astype(np.float32)
    shared = {
        "ident": ident, "tri": tri,
        "w_ada": np.ascontiguousarray(np.asarray(inp["w_ada"], np.float32)[0]),
        "w_in": np.ascontiguousarray(np.asarray(inp["w_in"], np.float32)[0]),
        "w_rg_a": np.ascontiguousarray(np.asarray(inp["w_rg_a"], np.float32)[0]),
        "w_rg_x": np.ascontiguousarray(np.asarray(inp["w_rg_x"], np.float32)[0]),
        "w_out": np.ascontiguousarray(np.asarray(inp["w_out"], np.float32)[0]),
        "w_ffn_in": np.ascontiguousarray(np.asarray(inp["w_ffn_in"], np.float32)[0]),
        "w_ffn_out": np.ascontiguousarray(np.asarray(inp["w_ffn_out"], np.float32)[0]),
    }
    maps = []
    for core in range(8):
        b, h = core // 2, core % 2
        if h == 1:
            xwin = np.ascontiguousarray(x[b])
            kb = np.zeros((128, W), np.float32)
        else:
            xwin = np.concatenate([np.zeros((2048, D), np.float32), x[b, :2048]], axis=0)
            kb = np.zeros((128, W), np.float32)
            kb[:, :2048] = NEG
        m = dict(shared)
        m["xw"] = xwin
        m["kbias"] = kb
        m["vecs"] = _pack_vecs(inp, c[b], float(h))
        maps.append(m)
    return maps


def kernel(**inputs):
    nc, T = build()
    in_maps = make_in_maps(inputs)
    res = run_bass_kernel_spmd(nc, in_maps, core_ids=list(range(8)))
    out = np.zeros((4, 4096, D), np.float32)
    for core in range(8):
        b, h = core // 2, core % 2
        out[b, h * 2048:(h + 1) * 2048] = np.asarray(res.results[core]["out"], np.float32)
    return out
```
